# Optimizing a Trainium2 kernel written in Bass

```python
import math
import jax
import jax.numpy as jnp
from jax import lax
import numpy as np

D_MODEL = 4096
BATCH = 4
SEQ = 2048
DEPTH = 2

GRID_W = 64
CTX_LEN = 256
EPS = 1e-6

A_HEADS = 16
A_Q_LORA = 1024
A_KV_LORA = 512
A_NOPE = 128
A_ROPE = 64
A_VDIM = 128
A_WIDTH = A_HEADS * A_VDIM
A_COLS = A_Q_LORA + A_KV_LORA + A_ROPE
ROPE_THETA = 10000.0
Q_BLOCK = 128

B_HEADS = 16
B_HDIM = 64
B_WIDTH = B_HEADS * B_HDIM
B_DECAY_LORA = 128
B_AAA_LORA = 128
B_GATE_LORA = 256
B_COLS = 3 * B_WIDTH + B_DECAY_LORA + B_AAA_LORA + B_GATE_LORA
B_SPLITS = (B_WIDTH, 2 * B_WIDTH, 3 * B_WIDTH, 3 * B_WIDTH + B_DECAY_LORA, 3 * B_WIDTH + B_DECAY_LORA + B_AAA_LORA)
B_GN_EPS = B_HDIM * 1e-5

C_HEADS = 8
C_HDIM = 128
C_WIDTH = C_HEADS * C_HDIM
C_CONV = 5
C_CHUNK = 64
C_COLS = 4 * C_WIDTH + 4 * C_HEADS

G_COLS = 3 * D_MODEL
IN_COLS = A_COLS + B_COLS + C_COLS + G_COLS
IN_SPLITS = (A_COLS, A_COLS + B_COLS, A_COLS + B_COLS + C_COLS)

N_EXPERTS = 32
TOP_K = 4
E_FF = D_MODEL // 8
SWIGLU_LIMIT = 7.0
SWIGLU_ALPHA = 1.702

kernel_name = 'hybrid_mla_rwkv7_gdn_moe_dit_block'


def rms_norm(x, g):
    xf = x.astype(jnp.float32)
    y = xf * lax.rsqrt(jnp.mean(xf * xf, axis=-1, keepdims=True) + EPS)
    return (y * g.astype(jnp.float32)).astype(x.dtype)


def modulate(h, shift, scale):
    return h * (1.0 + scale) + shift


def l2_normalize(t):
    return t * lax.rsqrt(jnp.sum(t * t, axis=-1, keepdims=True) + EPS)


def heads_l2(t, hd):
    sh = t.shape
    return l2_normalize(t.reshape(sh[:-1] + (sh[-1] // hd, hd))).reshape(sh)


def centred_shift(p):
    prev = jnp.pad(p[:, :-1], ((0, 0), (1, 0), (0, 0)))
    nxt = jnp.pad(p[:, 1:], ((0, 0), (0, 1), (0, 0)))
    return 0.5 * (prev + nxt)


def centred_dwconv(p, w):
    k = w.shape[0]
    r = k // 2
    t = p.shape[1]
    pp = jnp.pad(p, ((0, 0), (r, r), (0, 0)))
    out = pp[:, 0:t] * w[0]
    for i in range(1, k):
        out = out + pp[:, i:i + t] * w[i]
    return out


def bidir(t_f, t_b):
    return jnp.concatenate([t_f, jnp.flip(t_b, axis=1)], axis=0)


def merge_dirs(y):
    half = y.shape[0] // 2
    return y[:half] + jnp.flip(y[half:], axis=1)


def axial_rope_tables(n):
    rows = n // GRID_W
    row = jnp.repeat(jnp.arange(rows, dtype=jnp.float32), GRID_W)
    col = jnp.tile(jnp.arange(GRID_W, dtype=jnp.float32), rows)
    half = A_ROPE // 2
    freq = ROPE_THETA ** (-jnp.arange(0, half, 2, dtype=jnp.float32) / half)
    ang_r = row[:, None] * freq[None, :]
    ang_c = col[:, None] * freq[None, :]
    return (jnp.cos(ang_r), jnp.sin(ang_r), jnp.cos(ang_c), jnp.sin(ang_c))


def rotate_pairs(x, cos, sin):
    m = x.shape[-1] // 2
    x1, x2 = x[..., :m], x[..., m:]
    return jnp.concatenate([x1 * cos - x2 * sin, x1 * sin + x2 * cos], axis=-1)


def axial_rope(x, tables):
    extra = (1,) * (x.ndim - 3)
    cr, sr, cc, sc = [t.reshape((t.shape[0],) + extra + (t.shape[1],)) for t in tables]
    h = x.shape[-1] // 2
    xf = x.astype(jnp.float32)
    y = jnp.concatenate([rotate_pairs(xf[..., :h], cr, sr), rotate_pairs(xf[..., h:], cc, sc)], axis=-1)
    return y.astype(x.dtype)


def mla_project(pa, qnorm_g, wqb, kvnorm_g, wkvb):
    bsz, t = pa.shape[:2]
    cq, ckv, kpe = jnp.split(pa, (A_Q_LORA, A_Q_LORA + A_KV_LORA), axis=-1)
    q = (rms_norm(cq, qnorm_g) @ wqb).reshape(bsz, t, A_HEADS, A_NOPE + A_ROPE)
    kv = (rms_norm(ckv, kvnorm_g) @ wkvb).reshape(bsz, t, A_HEADS, A_NOPE + A_VDIM)
    return q[..., :A_NOPE], q[..., A_NOPE:], kv[..., :A_NOPE], kv[..., A_NOPE:], kpe


def mla_heads(q_nope, q_pe, k_nope, k_pe):
    q = jnp.concatenate([q_nope, q_pe], axis=-1)
    k_pe_h = jnp.broadcast_to(k_pe[:, :, None, :], k_nope.shape[:-1] + (A_ROPE,))
    k = jnp.concatenate([k_nope, k_pe_h], axis=-1)
    return q, k


def softmax_attend(q, k, v):
    s = jnp.einsum('bqhd,bkhd->bhqk', q, k).astype(jnp.float32) * (A_NOPE + A_ROPE) ** -0.5
    p = jax.nn.softmax(s, axis=-1).astype(v.dtype)
    return jnp.einsum('bhqk,bkhd->bqhd', p, v)


def mla_mixer(pa_c, pa_l, qnorm_g, wqb, kvnorm_g, wkvb, rope):
    qn_c, qp_c, kn_c, v_c, kp_c = mla_project(pa_c, qnorm_g, wqb, kvnorm_g, wkvb)
    qn_l, qp_l, kn_l, v_l, kp_l = mla_project(pa_l, qnorm_g, wqb, kvnorm_g, wkvb)
    q_c, k_c = mla_heads(qn_c, qp_c, kn_c, kp_c)
    q_l, k_l = mla_heads(qn_l, axial_rope(qp_l, rope), kn_l, axial_rope(kp_l, rope))
    bsz, t_c = pa_c.shape[:2]
    n = pa_l.shape[1]
    o_c = softmax_attend(q_c, k_c, v_c).reshape(bsz, t_c, A_WIDTH)
    k_all = jnp.concatenate([k_c, k_l], axis=1)
    v_all = jnp.concatenate([v_c, v_l], axis=1)
    q_blocks = jnp.moveaxis(q_l.reshape(bsz, n // Q_BLOCK, Q_BLOCK, A_HEADS, A_NOPE + A_ROPE), 1, 0)
    o_l = lax.map(lambda qb: softmax_attend(qb, k_all, v_all), q_blocks)
    o_l = jnp.moveaxis(o_l, 0, 1).reshape(bsz, n, A_WIDTH)
    return o_c, o_l


def rwkv_prepare(pb, mu, w0, w2, a0, a2, g2, k_k, k_a):
    p = (pb + mu * (centred_shift(pb) - pb)).astype(jnp.float32)
    r, k, v, wl, al, gl = jnp.split(p, B_SPLITS, axis=-1)
    w_log = -jax.nn.softplus(-(w0[:, None, None, :] + jnp.einsum('btr,drc->dbtc', jnp.tanh(wl), w2))) - 0.5
    decay = jnp.exp(-jnp.exp(w_log))
    a = jax.nn.sigmoid(a0[:, None, None, :] + jnp.einsum('btr,drc->dbtc', al, a2))
    kk = heads_l2(k * k_k, B_HDIM)
    kd = k[None] * (1.0 + (a - 1.0) * k_a)
    g = jax.nn.sigmoid(gl) @ g2
    return r, v, kk, decay, a, kd, g


def rwkv_scan(r, w, k, v, kk, a, s0):
    def heads(t):
        return jnp.moveaxis(t.reshape(t.shape[0], t.shape[1], B_HEADS, B_HDIM), 1, 0)

    def step(s, inp):
        r_t, w_t, k_t, v_t, kk_t, a_t = inp
        sa = jnp.einsum('xhij,xhj->xhi', s, -kk_t)
        s = (s * w_t[:, :, None, :] + sa[..., None] * (kk_t * a_t)[:, :, None, :]
             + v_t[..., None] * k_t[:, :, None, :])
        return s, jnp.einsum('xhij,xhj->xhi', s, r_t)

    s, y = lax.scan(step, s0, tuple(heads(t) for t in (r, w, k, v, kk, a)))
    return jnp.moveaxis(y, 0, 1), s


def rwkv_mixer(pb_c, pb_l, mu, w0, w2, a0, a2, g2, k_k, k_a, r_k, lnx_g, lnx_b):
    bsz = pb_l.shape[0]

    def run(pb, s0):
        r, v, kk, decay, a, kd, g = rwkv_prepare(pb, mu, w0, w2, a0, a2, g2, k_k, k_a)
        y, s = rwkv_scan(bidir(r, r), bidir(decay[0], decay[1]), bidir(kd[0], kd[1]),
                         bidir(v, v), bidir(kk, kk), bidir(a[0], a[1]), s0)
        y = merge_dirs(y)
        t = y.shape[1]
        mean = jnp.mean(y, axis=-1, keepdims=True)
        var = jnp.mean(jnp.square(y - mean), axis=-1, keepdims=True)
        yn = ((y - mean) * lax.rsqrt(var + B_GN_EPS)).reshape(bsz, t, B_WIDTH) * lnx_g + lnx_b
        rh = r.reshape(bsz, t, B_HEADS, B_HDIM)
        kdh = kd.reshape(2, bsz, t, B_HEADS, B_HDIM)
        bonus = jnp.einsum('bthn,dbthn->bth', rh * r_k, kdh)[..., None] * v.reshape(bsz, t, B_HEADS, B_HDIM)
        out = (yn + bonus.reshape(bsz, t, B_WIDTH)) * g
        return out.astype(pb.dtype), s

    s0 = jnp.zeros((2 * bsz, B_HEADS, B_HDIM, B_HDIM), jnp.float32)
    y_c, s_c = run(pb_c, s0)
    y_l, _ = run(pb_l, s_c)
    return y_c, y_l


def gdn_prepare(pc, conv_w, a_log, dt_bias):
    bsz, t = pc.shape[:2]
    qkv, z, alpha, beta = jnp.split(pc, (3 * C_WIDTH, 4 * C_WIDTH, 4 * C_WIDTH + 2 * C_HEADS), axis=-1)
    qkv = jax.nn.silu(centred_dwconv(qkv, conv_w).astype(jnp.float32)).reshape(bsz, t, 3, C_HEADS, C_HDIM)
    q = l2_normalize(qkv[:, :, 0]) * C_HDIM ** -0.5
    k = l2_normalize(qkv[:, :, 1])
    v = qkv[:, :, 2]
    g = -jnp.exp(a_log) * jax.nn.softplus(alpha.astype(jnp.float32).reshape(bsz, t, 2, C_HEADS) + dt_bias)
    b = jax.nn.sigmoid(beta.astype(jnp.float32).reshape(bsz, t, 2, C_HEADS))
    return q, k, v, g, b, z


def gdn_chunked(q, k, v, g, beta, s0):
    x_, t, h = g.shape
    dv = v.shape[-1]
    nc = t // C_CHUNK

    def chunks(a):
        a = a.reshape((x_, nc, C_CHUNK, h) + a.shape[3:])
        return jnp.moveaxis(a, (1, 3), (0, 2))

    qc, kc, vc, gc, bc = [chunks(a) for a in (q, k, v, g, beta)]
    gcum = jnp.cumsum(gc, axis=-1)
    idx = jnp.arange(C_CHUNK)
    incl = idx[:, None] >= idx[None, :]
    strict = idx[:, None] > idx[None, :]
    diff = gcum[..., :, None] - gcum[..., None, :]
    decay = jnp.where(incl, jnp.exp(jnp.where(incl, diff, 0.0)), 0.0)
    kb = kc * bc[..., None]
    lmat = jnp.where(strict, jnp.einsum('nxhid,nxhjd->nxhij', kb, kc) * decay, 0.0)
    amat = lmat + jnp.eye(C_CHUNK, dtype=lmat.dtype)
    rhs = jnp.concatenate([vc * bc[..., None], kb * jnp.exp(gcum)[..., None]], axis=-1)
    sol = lax.linalg.triangular_solve(amat, rhs, left_side=True, lower=True, unit_diagonal=True)
    u, w = sol[..., :dv], sol[..., dv:]
    intra = jnp.where(incl, jnp.einsum('nxhid,nxhjd->nxhij', qc, kc) * decay, 0.0)

    def step(s, inp):
        q_i, k_i, u_i, w_i, a_i, g_i = inp
        v_new = u_i - jnp.einsum('xhck,xhkv->xhcv', w_i, s)
        o = (jnp.einsum('xhck,xhkv->xhcv', q_i * jnp.exp(g_i)[..., None], s)
             + jnp.einsum('xhij,xhjv->xhiv', a_i, v_new))
        g_last = g_i[..., -1:]
        s = (s * jnp.exp(g_last)[..., None]
             + jnp.einsum('xhck,xhcv->xhkv', k_i * jnp.exp(g_last - g_i)[..., None], v_new))
        return s, o

    s, o = lax.scan(step, s0, (qc, kc, u, w, intra, gcum))
    o = jnp.moveaxis(o, (0, 2), (1, 3)).reshape(x_, t, h, dv)
    return o, s


def gdn_mixer(pc_c, pc_l, conv_w, a_log, dt_bias, norm_g):
    bsz = pc_l.shape[0]

    def run(pc, s0):
        q, k, v, g, b, z = gdn_prepare(pc, conv_w, a_log, dt_bias)
        o, s = gdn_chunked(bidir(q, q), bidir(k, k), bidir(v, v),
                           bidir(g[:, :, 0], g[:, :, 1]), bidir(b[:, :, 0], b[:, :, 1]), s0)
        o = merge_dirs(o)
        out = rms_norm(o, norm_g) * jax.nn.silu(z.astype(jnp.float32).reshape(o.shape))
        return out.reshape(pc.shape[0], pc.shape[1], C_WIDTH).astype(pc.dtype), s

    s0 = jnp.zeros((2 * bsz, C_HEADS, C_HDIM, C_HDIM), jnp.float32)
    y_c, s_c = run(pc_c, s0)
    y_l, _ = run(pc_l, s_c)
    return y_c, y_l


def merge_branches(ya, yb, yc, gates, w_up_a, w_up_b, w_up_c, w_out):
    ga, gb, gc = jnp.split(jax.nn.sigmoid(gates), 3, axis=-1)
    mixed = ga * (ya @ w_up_a) + gb * (yb @ w_up_b) + gc * (yc @ w_up_c)
    return mixed @ w_out


def moe_ffn(h, router_w, router_b, w1, b1, w2, b2):
    shp = h.shape
    ht = h.reshape(-1, shp[-1])
    logits = (ht @ router_w + router_b).astype(jnp.float32)
    top_val, top_idx = lax.top_k(logits, TOP_K)
    wts = jax.nn.softmax(top_val, axis=-1)
    combine = jnp.sum(jax.nn.one_hot(top_idx, N_EXPERTS, dtype=jnp.float32) * wts[..., None], axis=1)
    out = jnp.zeros(ht.shape, jnp.float32)
    for e in range(N_EXPERTS):
        gu = ht @ w1[e] + b1[e]
        gate = jnp.minimum(gu[:, :E_FF], SWIGLU_LIMIT)
        up = jnp.clip(gu[:, E_FF:], -SWIGLU_LIMIT, SWIGLU_LIMIT)
        act = gate * jax.nn.sigmoid(SWIGLU_ALPHA * gate) * (up + 1.0)
        out = out + combine[:, e:e + 1] * (act @ w2[e] + b2[e])
    return out.astype(h.dtype).reshape(shp)


def setup_inputs(seed: int = 0) -> dict:
    key = jax.random.key(seed)
    keys = iter(jax.random.split(key, 48))

    def normal(shape, scale):
        return jax.random.normal(next(keys), shape, jnp.float32) * scale

    def uniform(shape, lo, hi):
        return jax.random.uniform(next(keys), shape, jnp.float32, lo, hi)

    def gain(shape):
        return 1.0 + normal(shape, 0.05)

    L = DEPTH
    dt = jnp.exp(uniform((L, 2, C_HEADS), math.log(1e-3), math.log(1e-1)))
    return {
        'x': normal((BATCH, SEQ, D_MODEL), 1.0),
        'c': normal((BATCH, D_MODEL), 1.0),
        'ctx': normal((BATCH, CTX_LEN, D_MODEL), 1.0),
        'c_ctx': normal((D_MODEL,), 1.0),
        'ada_w': normal((L, D_MODEL, 6 * D_MODEL), 0.5 * D_MODEL ** -0.5),
        'ada_b': normal((L, 6 * D_MODEL), 0.02),
        'norm1_g': gain((L, D_MODEL)),
        'norm2_g': gain((L, D_MODEL)),
        'w_in': normal((L, D_MODEL, IN_COLS), D_MODEL ** -0.5),
        'mla_qnorm_g': gain((L, A_Q_LORA)),
        'mla_wqb': normal((L, A_Q_LORA, A_HEADS * (A_NOPE + A_ROPE)), A_Q_LORA ** -0.5),
        'mla_kvnorm_g': gain((L, A_KV_LORA)),
        'mla_wkvb': normal((L, A_KV_LORA, A_HEADS * (A_NOPE + A_VDIM)), A_KV_LORA ** -0.5),
        'rwkv_mu': uniform((L, B_COLS), 0.0, 1.0),
        'rwkv_w0': normal((L, 2, B_WIDTH), 0.5),
        'rwkv_w2': normal((L, 2, B_DECAY_LORA, B_WIDTH), 0.5 * B_DECAY_LORA ** -0.5),
        'rwkv_a0': normal((L, 2, B_WIDTH), 0.1),
        'rwkv_a2': normal((L, 2, B_AAA_LORA, B_WIDTH), 0.5 * B_AAA_LORA ** -0.5),
        'rwkv_g2': normal((L, B_GATE_LORA, B_WIDTH), B_GATE_LORA ** -0.5),
        'rwkv_kk': 0.85 + normal((L, B_WIDTH), 0.05),
        'rwkv_ka': gain((L, B_WIDTH)),
        'rwkv_rk': normal((L, B_HEADS, B_HDIM), 0.1),
        'rwkv_lnx_g': gain((L, B_WIDTH)),
        'rwkv_lnx_b': normal((L, B_WIDTH), 0.01),
        'gdn_conv': normal((L, C_CONV, 3 * C_WIDTH), C_CONV ** -0.5),
        'gdn_alog': jnp.log(uniform((L, 2, C_HEADS), 1.0, 16.0)),
        'gdn_dtb': dt + jnp.log(-jnp.expm1(-dt)),
        'gdn_norm_g': gain((L, C_HDIM)),
        'w_up_a': normal((L, A_WIDTH, D_MODEL), A_WIDTH ** -0.5),
        'w_up_b': normal((L, B_WIDTH, D_MODEL), B_WIDTH ** -0.5),
        'w_up_c': normal((L, C_WIDTH, D_MODEL), C_WIDTH ** -0.5),
        'w_out': normal((L, D_MODEL, D_MODEL), D_MODEL ** -0.5),
        'router_w': normal((L, D_MODEL, N_EXPERTS), D_MODEL ** -0.5),
        'router_b': normal((L, N_EXPERTS), 0.01),
        'exp_w1': normal((L, N_EXPERTS, D_MODEL, 2 * E_FF), D_MODEL ** -0.5),
        'exp_b1': normal((L, N_EXPERTS, 2 * E_FF), 0.01),
        'exp_w2': normal((L, N_EXPERTS, E_FF, D_MODEL), E_FF ** -0.5),
        'exp_b2': normal((L, N_EXPERTS, D_MODEL), 0.01),
        'final_norm_g': gain((D_MODEL,)),
    }


def reference(x, c, ctx, c_ctx, ada_w, ada_b, norm1_g, norm2_g, w_in,
              mla_qnorm_g, mla_wqb, mla_kvnorm_g, mla_wkvb,
              rwkv_mu, rwkv_w0, rwkv_w2, rwkv_a0, rwkv_a2, rwkv_g2, rwkv_kk, rwkv_ka, rwkv_rk,
              rwkv_lnx_g, rwkv_lnx_b,
              gdn_conv, gdn_alog, gdn_dtb, gdn_norm_g,
              w_up_a, w_up_b, w_up_c, w_out,
              router_w, router_b, exp_w1, exp_b1, exp_w2, exp_b2, final_norm_g):
    n = x.shape[1]
    rope = axial_rope_tables(n)
    xl, xc = x, ctx
    for l in range(DEPTH):
        last = l == DEPTH - 1
        ml = jnp.split(jax.nn.silu(c) @ ada_w[l] + ada_b[l], 6, axis=-1)
        mc = jnp.split(jax.nn.silu(c_ctx) @ ada_w[l] + ada_b[l], 6, axis=-1)
        sh1l, sc1l, g1l, sh2l, sc2l, g2l = [m[:, None, :] for m in ml]
        sh1c, sc1c, g1c, sh2c, sc2c, g2c = mc

        pl = modulate(rms_norm(xl, norm1_g[l]), sh1l, sc1l) @ w_in[l]
        pc = modulate(rms_norm(xc, norm1_g[l]), sh1c, sc1c) @ w_in[l]
        pa_l, pb_l, pg_l, gate_l = jnp.split(pl, IN_SPLITS, axis=-1)
        pa_c, pb_c, pg_c, gate_c = jnp.split(pc, IN_SPLITS, axis=-1)
        ya_c, ya_l = mla_mixer(pa_c, pa_l, mla_qnorm_g[l], mla_wqb[l], mla_kvnorm_g[l], mla_wkvb[l], rope)
        yb_c, yb_l = rwkv_mixer(pb_c, pb_l, rwkv_mu[l], rwkv_w0[l], rwkv_w2[l], rwkv_a0[l], rwkv_a2[l],
                                rwkv_g2[l], rwkv_kk[l], rwkv_ka[l], rwkv_rk[l], rwkv_lnx_g[l], rwkv_lnx_b[l])
        yc_c, yc_l = gdn_mixer(pg_c, pg_l, gdn_conv[l], gdn_alog[l], gdn_dtb[l], gdn_norm_g[l])
        proj = (w_up_a[l], w_up_b[l], w_up_c[l], w_out[l])
        moe_w = (router_w[l], router_b[l], exp_w1[l], exp_b1[l], exp_w2[l], exp_b2[l])

        xl = xl + g1l * merge_branches(ya_l, yb_l, yc_l, gate_l, *proj)
        xl = xl + g2l * moe_ffn(modulate(rms_norm(xl, norm2_g[l]), sh2l, sc2l), *moe_w)
        if not last:
            xc = xc + g1c * merge_branches(ya_c, yb_c, yc_c, gate_c, *proj)
            xc = xc + g2c * moe_ffn(modulate(rms_norm(xc, norm2_g[l]), sh2c, sc2c), *moe_w)
    return rms_norm(xl, final_norm_g)
```

```python
import math
from contextlib import ExitStack
import numpy as np
import concourse.bass as bass
import concourse.mybir as mybir
from concourse.bass_utils import run_bass_kernel_spmd

F32, BF16 = mybir.dt.float32, mybir.dt.bfloat16
AF = mybir.ActivationFunctionType
ALU = mybir.AluOpType
AX = mybir.AxisListType

D = 4096
DEPTH = 2
GRID_W = 64
EPS = 1e-6
A_HEADS, A_Q_LORA, A_KV_LORA, A_NOPE, A_ROPE, A_VDIM = 16, 1024, 512, 128, 64, 128
A_WIDTH = A_HEADS * A_VDIM
A_COLS = A_Q_LORA + A_KV_LORA + A_ROPE
B_HEADS, B_HDIM = 16, 64
B_WIDTH = 1024
B_COLS = 3 * B_WIDTH + 128 + 128 + 256
B_GN_EPS = B_HDIM * 1e-5
C_HEADS, C_HDIM = 8, 128
C_WIDTH = 1024
C_COLS = 4 * C_WIDTH + 4 * C_HEADS
IN_COLS = A_COLS + B_COLS + C_COLS + 3 * D
OFF_B = A_COLS
OFF_C = A_COLS + B_COLS
OFF_G = A_COLS + B_COLS + C_COLS
N_EXP, TOP_K, E_FF = 32, 4, 512
LIMIT, ALPHA = 7.0, 1.702
ROPE_THETA = 10000.0

VEC_SPEC = [("ada_b", 192), ("norm1_g", 32), ("norm2_g", 32), ("qnorm_g", 8), ("kvnorm_g", 4),
            ("mu", 28), ("w0", 16), ("a0", 16), ("k_k", 8), ("k_a", 8), ("r_k", 8), ("lnx_g", 8),
            ("lnx_b", 8), ("conv", 24 * 5), ("alog", 1), ("dtb", 1), ("gnorm_g", 1), ("b1", 32 * 8)]
VEC_OFF = {}
_o = 0
for _n, _c in VEC_SPEC:
    VEC_OFF[_n] = (_o, _c)
    _o += _c
NV = _o


class Sem:
    def __init__(self, h):
        self.h = h
        self.val = 0


class Buf:
    def __init__(self, name):
        self.name = name
        self.w = None
        self.rs = {}
        self.dsem = None


class Tile:
    def __init__(self, t, name):
        self.t = t
        self.b = Buf(name)

    def __getitem__(self, k):
        return self.t[k]


class Eng:
    def __init__(self, name, e, sem, self_wait):
        self.name, self.e, self.sem, self.self_wait = name, e, sem, self_wait
        self.waited = {}


class KB:
    def __init__(self, nc, n_dsem=60):
        self.nc = nc
        mk = lambda n: Sem(nc.alloc_semaphore(n))
        self.E = {
            "pe": Eng("pe", nc.tensor, mk("s_pe"), False),
            "dve": Eng("dve", nc.vector, mk("s_dve"), True),
            "act": Eng("act", nc.scalar, mk("s_act"), True),
            "pool": Eng("pool", nc.gpsimd, mk("s_pool"), True),
            "sp": Eng("sp", nc.sync, mk("s_sp"), False),
        }
        self.dfree = [mk(f"s_d{i}") for i in range(n_dsem)]
        self.dused = []
        self.n_inst = 0

    def _wait(self, E, ev):
        sem, val = ev
        if sem is E.sem and not E.self_wait:
            return
        if E.waited.get(id(sem), 0) >= val:
            return
        E.e.wait_ge(sem.h, val)
        E.waited[id(sem)] = val
        self.n_inst += 1

    def _deps(self, E, reads, writes):
        for t in reads:
            if t.b.w is not None:
                self._wait(E, t.b.w)
        for t in writes:
            if t.b.w is not None:
                self._wait(E, t.b.w)
            for ev in t.b.rs.values():
                if ev[0] is E.sem:
                    continue
                self._wait(E, ev)

    def _commit(self, ev, reads, writes):
        for t in reads:
            t.b.rs[id(ev[0])] = ev
        for t in writes:
            t.b.w = ev
            t.b.rs = {}

    def op(self, eng, fn, reads=(), writes=()):
        E = self.E[eng]
        self._deps(E, reads, writes)
        inst = fn(E.e)
        E.sem.val += 1
        inst.then_inc(E.sem.h, 1)
        self._commit((E.sem, E.sem.val), reads, writes)
        self.n_inst += 1
        return inst

    def mm(self, out_ap, lhsT, rhs, start, stop, reads, writes):
        return self.op("pe", lambda e: e.matmul(out_ap, lhsT, rhs, start=start, stop=stop), reads, writes)

    def dma(self, q, out_ap, in_ap, reads, writes, sb):
        E = self.E[q]
        if sb.b.dsem is None:
            sb.b.dsem = self.dfree.pop()
            self.dused.append(sb.b)
        ds = sb.b.dsem
        self._deps(E, reads, writes)
        if ds.val > 0:
            self._wait(E, (ds, ds.val))
        inst = E.e.dma_start(out=out_ap, in_=in_ap)
        ds.val += 16
        inst.then_inc(ds.h, 16)
        self._commit((ds, ds.val), reads, writes)
        self.n_inst += 1
        return inst

    def barrier(self):
        sp = self.E["sp"]
        for E in self.E.values():
            if E is not sp and E.sem.val > 0:
                self._wait(sp, (E.sem, E.sem.val))
        for b in self.dused:
            if b.dsem.val > 0:
                self._wait(sp, (b.dsem, b.dsem.val))
        inst = sp.e.nop()
        sp.sem.val += 1
        inst.then_inc(sp.sem.h, 1)
        for E in self.E.values():
            if E is not sp:
                self._wait(E, (sp.sem, sp.sem.val))
        for b in self.dused:
            self.dfree.append(b.dsem)
            b.dsem = None
        self.dused = []

    def final_wait(self):
        self.barrier()


class Phase:
    def __init__(self, kb):
        self.kb = kb
        self.es = ExitStack()
        self.n = 0
        Phase._uid = getattr(Phase, "_uid", 0) + 1
        self.uid = Phase._uid

    def __enter__(self):
        self.es.__enter__()
        return self

    def __exit__(self, *a):
        self.kb.barrier()
        return self.es.__exit__(*a)

    def sb(self, shape, dt, name=None):
        self.n += 1
        name = name or f"t{self.n}"
        t = self.es.enter_context(self.kb.nc.sbuf_tensor(f"{name}_{self.uid}_{self.n}", list(shape), dt))
        return Tile(t, name)

    def ps(self, shape, dt=F32, name=None):
        self.n += 1
        name = name or f"p{self.n}"
        t = self.es.enter_context(self.kb.nc.psum_tensor(f"{name}_{self.uid}_{self.n}", list(shape), dt))
        return Tile(t, name)


def token_tiles(TC, TL, mx=512):
    out = []
    s = 0
    while s < TC:
        n = min(mx, TC - s)
        out.append((s, n, False))
        s += n
    while s < TC + TL:
        n = min(mx, TC + TL - s)
        out.append((s, n, True))
        s += n
    return out


class Model:
    def __init__(self, TC, TL, dbg=None):
        self.TC, self.TL, self.T = TC, TL, TC + TL
        self.tiles = token_tiles(TC, TL)
        self.nc = bass.Bass("TRN2", target_bir_lowering=False)
        self.kb = KB(self.nc)
        self.dbg = dbg or {}
        self.ins = {}
        self.outs = {}
        self.scr = {}
        nc = self.nc
        self.vecs = Tile(nc.alloc_sbuf_tensor("vecs_sb", [128, DEPTH, NV], F32), "vecs_sb")
        self.mod = [Tile(nc.alloc_sbuf_tensor(f"mod_sb{l}", [128, 192, 2], F32), f"mod{l}") for l in range(DEPTH)]
        self.vecs_loaded = False

    def din(self, name, shape, dt=F32):
        if name not in self.ins:
            self.ins[name] = Tile(self.nc.dram_tensor(name, list(shape), dt, kind="ExternalInput").ap(), name)
        return self.ins[name]

    def dscr(self, name, shape, dt=F32):
        if name not in self.scr:
            kind = {"in": "ExternalInput", "out": "ExternalOutput"}.get(self.dbg.get(name), "Internal")
            t = Tile(self.nc.dram_tensor(name, list(shape), dt, kind=kind).ap(), name)
            self.scr[name] = t
            if kind == "ExternalInput":
                self.ins[name] = t
            if kind == "ExternalOutput":
                self.outs[name] = t
        return self.scr[name]

    def dout(self, name, shape, dt=F32):
        t = Tile(self.nc.dram_tensor(name, list(shape), dt, kind="ExternalOutput").ap(), name)
        self.outs[name] = t
        return t

    def vec(self, l, name):
        o, c = VEC_OFF[name]
        return self.vecs.t[:, l, o:o + c]

    def load_vecs(self):
        if self.vecs_loaded:
            return
        v = self.din("vecs", [128, DEPTH, NV])
        self.kb.dma("sp", self.vecs[:], v[:], [v], [self.vecs], sb=self.vecs)
        self.vecs_loaded = True

    def phase_mod(self, l):
        kb = self.kb
        self.load_vecs()
        csT = self.din("csT", [128, 32, 2])
        adaw_t = self.din("ada_w", [DEPTH, D, 6 * D])
        adaw = adaw_t.t[l].rearrange("(kc p) c -> p kc c", p=128)
        with Phase(kb) as ph:
            cs = ph.sb([128, 32, 2], F32)
            sc = ph.sb([128, 32, 2], F32)
            kb.dma("sp", cs[:], csT[:], [csT], [cs], sb=cs)
            kb.op("act", lambda e: e.activation(out=sc[:], in_=cs[:], func=AF.Silu), [cs], [sc])
            ps = ph.ps([128, 384])
            wts = [ph.sb([128, 32, 512], F32) for _ in range(2)]
            for ct in range(48):
                wt = wts[ct % 2]
                kb.dma("sp", wt[:], adaw[:, :, ct * 512:(ct + 1) * 512], [adaw_t], [wt], sb=wt)
                for ms in range(4):
                    j = ct * 4 + ms
                    for kc in range(32):
                        kb.mm(ps[:, 2 * j:2 * j + 2], wt[:, kc, ms * 128:(ms + 1) * 128], sc[:, kc, :],
                              kc == 0, kc == 31, [wt, sc], [ps])
            mod = self.mod[l]
            ab = self.vec(l, "ada_b")
            kb.op("dve", lambda e: e.tensor_tensor(
                out=mod[:], in0=ps[:].rearrange("p (j r) -> p j r", r=2),
                in1=ab.unsqueeze(2).to_broadcast([128, 192, 2]), op=ALU.add), [ps, self.vecs], [mod])

    def rms_stats(self, ph, src_fn, nk, n, dim, ones, sq, ps_ss, rstd, reads):
        kb = self.kb
        for kc in range(nk):
            s_ = sq[kc % len(sq)]
            kb.op("act", lambda e: e.activation(out=s_[:, :n], in_=src_fn(kc), func=AF.Square), reads, [s_])
            kb.mm(ps_ss[:, :n], ones[:], s_[:, :n], kc == 0, kc == nk - 1, [ones, s_], [ps_ss])
        kb.op("act", lambda e: e.activation(out=rstd[:, :n], in_=ps_ss[:, :n], func=AF.Sqrt,
                                            bias=self.eps_t[:, 0:1], scale=1.0 / dim), [ps_ss, self.eps_t], [rstd])
        kb.op("dve", lambda e: e.reciprocal(out=rstd[:, :n], in_=rstd[:, :n]), [rstd], [rstd])

    def consts(self, ph):
        kb = self.kb
        self.ones = ph.sb([128, 128], F32, "ones")
        kb.op("pool", lambda e: e.memset(self.ones[:], 1.0), [], [self.ones])
        self.eps_t = ph.sb([128, 2], F32, "eps")
        kb.op("pool", lambda e: e.memset(self.eps_t[:, 0:1], EPS), [], [self.eps_t])
        kb.op("pool", lambda e: e.memset(self.eps_t[:, 1:2], B_GN_EPS), [], [self.eps_t])
        self.one_t = ph.sb([128, 1], F32, "one")
        kb.op("pool", lambda e: e.memset(self.one_t[:], 1.0), [], [self.one_t])

    def phase_proj(self, l):
        kb = self.kb
        self.load_vecs()
        T = self.T
        xs = self.dscr("xs", [D, T])
        pT = self.dscr("pT", [IN_COLS, T])
        win_t = self.din("w_in", [DEPTH, D, IN_COLS])
        win = win_t.t[l].rearrange("(kc p) c -> p kc c", p=128)
        xsv = xs.t.rearrange("(kc p) t -> p kc t", p=128)
        groups, cur, tot = [], [], 0
        for tl in self.tiles:
            if tot + tl[1] > 1280:
                groups.append(cur)
                cur, tot = [], 0
            cur.append(tl)
            tot += tl[1]
        groups.append(cur)
        mod = self.mod[l]
        with Phase(kb) as ph:
            self.consts(ph)
            gm = ph.sb([128, 32, 2], F32)
            g1 = self.vec(l, "norm1_g")
            kb.op("dve", lambda e: e.scalar_tensor_tensor(
                out=gm[:], in0=mod[:, 32:64, :], scalar=1.0, in1=g1.unsqueeze(2).to_broadcast([128, 32, 2]),
                op0=ALU.add, op1=ALU.mult), [mod, self.vecs], [gm])
            hT = ph.sb([128, 32, 1280], BF16)
            xt = ph.sb([128, 32, 256], F32)
            sq = [ph.sb([128, 512], F32) for _ in range(2)]
            rstd = ph.sb([128, 512], F32)
            tmp = [ph.sb([128, 512], F32) for _ in range(2)]
            wts = [ph.sb([128, 32, 512], BF16) for _ in range(2)]
            ots = [ph.sb([128, 512], F32) for _ in range(4)]
            ps_ss = ph.ps([128, 512])
            ps_mm = [ph.ps([128, 512]) for _ in range(6)]
            rot = 0
            wi = 0
            for grp in groups:
                off = 0
                for (t0, n, lat) in [(a + o, min(256, b_ - o), c_) for (a, b_, c_) in grp for o in range(0, b_, 256)]:
                    r = 0 if lat else 1
                    kb.dma("sp", xt[:, :, :n], xsv[:, :, t0:t0 + n], [xs], [xt], sb=xt)
                    self.rms_stats(ph, lambda kc: xt[:, kc, :n], 32, n, D, self.ones, sq, ps_ss, rstd, [xt])
                    for kc in range(32):
                        tp = tmp[kc % 2]
                        kb.op("dve", lambda e: e.scalar_tensor_tensor(
                            out=tp[:, :n], in0=xt[:, kc, :n], scalar=gm[:, kc, r:r + 1], in1=rstd[:, :n],
                            op0=ALU.mult, op1=ALU.mult), [xt, gm, rstd], [tp])
                        kb.op("act", lambda e: e.activation(
                            out=hT[:, kc, off:off + n], in_=tp[:, :n], func=AF.Identity,
                            bias=mod[:, kc, r:r + 1], scale=1.0), [tp, mod], [hT])
                    off += n
                c0 = 0
                while c0 < IN_COLS:
                    w = min(512, IN_COLS - c0)
                    wt = wts[wi % 2]
                    wi += 1
                    kb.dma("pool", wt[:, :, :w], win[:, :, c0:c0 + w], [win_t], [wt], sb=wt)
                    mo = 0
                    while mo < w:
                        msz = min(128, w - mo)
                        off = 0
                        for (t0, n, lat) in grp:
                            ps = ps_mm[rot % 6]
                            ot = ots[rot % 4]
                            for kc in range(32):
                                kb.mm(ps[:msz, :n], wt[:, kc, mo:mo + msz], hT[:, kc, off:off + n],
                                      kc == 0, kc == 31, [wt, hT], [ps])
                            if rot % 2 == 0:
                                kb.op("act", lambda e: e.copy(out=ot[:msz, :n], in_=ps[:msz, :n]), [ps], [ot])
                            else:
                                kb.op("dve", lambda e: e.tensor_copy(out=ot[:msz, :n], in_=ps[:msz, :n]), [ps], [ot])
                            kb.dma("sp", pT[c0 + mo:c0 + mo + msz, t0:t0 + n], ot[:msz, :n], [ot], [pT], sb=ot)
                            rot += 1
                            off += n
                        mo += msz
                    c0 += w


VEC_SRC = ["ada_b", "norm1_g", "norm2_g", "mla_qnorm_g", "mla_kvnorm_g", "rwkv_mu", "rwkv_w0", "rwkv_a0",
           "rwkv_kk", "rwkv_ka", "rwkv_rk", "rwkv_lnx_g", "rwkv_lnx_b", "gdn_conv", "gdn_alog", "gdn_dtb",
           "gdn_norm_g", "exp_b1"]


def _col(v, nch):
    return np.ascontiguousarray(np.asarray(v, np.float32).reshape(nch, 128).T)


def pack_vecs(p):
    out = np.zeros((128, DEPTH, NV), np.float32)
    for l in range(DEPTH):
        def put(name, arr):
            o, c = VEC_OFF[name]
            out[:arr.shape[0], l, o:o + c] = arr
        put("ada_b", _col(p["ada_b"][l], 192))
        put("norm1_g", _col(p["norm1_g"][l], 32))
        put("norm2_g", _col(p["norm2_g"][l], 32))
        put("qnorm_g", _col(p["mla_qnorm_g"][l], 8))
        put("kvnorm_g", _col(p["mla_kvnorm_g"][l], 4))
        put("mu", _col(p["rwkv_mu"][l], 28))
        put("w0", _col(p["rwkv_w0"][l].reshape(-1), 16))
        put("a0", _col(p["rwkv_a0"][l].reshape(-1), 16))
        put("k_k", _col(p["rwkv_kk"][l], 8))
        put("k_a", _col(p["rwkv_ka"][l], 8))
        put("r_k", _col(p["rwkv_rk"][l].reshape(-1), 8))
        put("lnx_g", _col(p["rwkv_lnx_g"][l], 8))
        put("lnx_b", _col(p["rwkv_lnx_b"][l], 8))
        cw = np.asarray(p["gdn_conv"][l], np.float32)
        put("conv", np.ascontiguousarray(cw.reshape(5, 24, 128).transpose(2, 1, 0)).reshape(128, 120))
        put("alog", np.asarray(p["gdn_alog"][l], np.float32).reshape(16, 1))
        put("dtb", np.asarray(p["gdn_dtb"][l], np.float32).reshape(16, 1))
        put("gnorm_g", np.asarray(p["gdn_norm_g"][l], np.float32).reshape(128, 1))
        b1 = np.asarray(p["exp_b1"][l], np.float32)
        put("b1", np.ascontiguousarray(b1.reshape(32, 8, 128).transpose(2, 0, 1)).reshape(128, 256))
    return out


def _evac(kb, i, out_ap, in_ap, reads, writes):
    if i % 2 == 0:
        kb.op("act", lambda e: e.copy(out=out_ap, in_=in_ap), reads, writes)
    else:
        kb.op("dve", lambda e: e.tensor_copy(out=out_ap, in_=in_ap), reads, writes)


def _subtiles(tiles, mx):
    return [(a + o, min(mx, n - o), lat) for (a, n, lat) in tiles for o in range(0, n, mx)]


def phase_mla(self, l, last):
    kb = self.kb
    self.load_vecs()
    T, TC, TL = self.T, self.TC, self.TL
    pT = self.dscr("pT", [IN_COLS, T])
    yaT = self.dscr("yaT", [A_WIDTH, T], BF16)
    wqb_t = self.din("mla_wqb", [DEPTH, A_Q_LORA, 3072])
    wkvb_t = self.din("mla_wkvb", [DEPTH, A_KV_LORA, 4096])
    ropeC_d = self.din("ropeC", [64, TL])
    ropeS_d = self.din("ropeS", [64, TL])
    ropeP_d = self.din("ropePT", [64, 64])
    wqb = wqb_t.t[l].rearrange("(kc p) c -> p kc c", p=128)
    wkvb = wkvb_t.t[l].rearrange("(kc p) c -> p kc c", p=128)
    scale = (A_NOPE + A_ROPE) ** -0.5
    NKC = T // 128
    with Phase(kb) as ph:
        self.consts(ph)
        ones_bf = ph.sb([128, 128], BF16)
        kb.op("pool", lambda e: e.memset(ones_bf[:], 1.0), [], [ones_bf])
        cqn = ph.sb([128, 8, T], BF16)
        ckvn = ph.sb([128, 4, T], BF16)
        st = ph.sb([128, 8, 256], F32)
        sq = [ph.sb([128, 512], F32) for _ in range(2)]
        rstd = ph.sb([128, 512], F32)
        ps_ss = ph.ps([128, 512])
        ps_a = [ph.ps([128, 512]) for _ in range(3)]
        ps_o = [ph.ps([128, 512]) for _ in range(2)]
        ps_l = [ph.ps([128, 512]) for _ in range(2)]
        qg = self.vec(l, "qnorm_g")
        kg = self.vec(l, "kvnorm_g")
        for (t0, n, lat) in _subtiles(self.tiles, 256):
            for (r0, nk, dim, g, dst) in ((0, 8, 1024, qg, cqn), (1024, 4, 512, kg, ckvn)):
                src = pT.t[r0:r0 + nk * 128, :].rearrange("(kc p) t -> p kc t", p=128)
                kb.dma("sp", st[:, :nk, :n], src[:, :, t0:t0 + n], [pT], [st], sb=st)
                self.rms_stats(ph, lambda kc: st[:, kc, :n], nk, n, dim, self.ones, sq, ps_ss, rstd, [st])
                for kc in range(nk):
                    kb.op("dve", lambda e: e.scalar_tensor_tensor(
                        out=dst[:, kc, t0:t0 + n], in0=st[:, kc, :n], scalar=g[:, kc:kc + 1], in1=rstd[:, :n],
                        op0=ALU.mult, op1=ALU.mult), [st, rstd, self.vecs], [dst])
        rC = ph.sb([64, TL], F32)
        rS = ph.sb([64, TL], F32)
        rP = ph.sb([64, 64], F32)
        kb.dma("sp", rC[:], ropeC_d[:], [ropeC_d], [rC], sb=rC)
        kb.dma("sp", rS[:], ropeS_d[:], [ropeS_d], [rS], sb=rS)
        kb.dma("sp", rP[:], ropeP_d[:], [ropeP_d], [rP], sb=rP)
        kpe = ph.sb([64, T], F32)
        kb.dma("sp", kpe[:], pT.t[1536:1600, :], [pT], [kpe], sb=kpe)
        krope = ph.sb([64, T], BF16)
        t1 = ph.sb([64, 512], F32)
        t2 = ph.sb([64, 512], F32)
        rot = 0

        def apply_rope(dst, src_ap, src_reads, rot_ps, t0, n, lat):
            if not lat:
                kb.op("act", lambda e: e.copy(out=dst[:, t0:t0 + n], in_=src_ap), src_reads, [dst])
                return
            p0 = t0 - TC
            kb.op("dve", lambda e: e.tensor_tensor(out=t1[:, :n], in0=src_ap, in1=rC[:, p0:p0 + n], op=ALU.mult),
                  src_reads + [rC], [t1])
            kb.op("dve", lambda e: e.tensor_tensor(out=t2[:, :n], in0=rot_ps[:64, :n], in1=rS[:, p0:p0 + n],
                                                   op=ALU.mult), [rot_ps, rS], [t2])
            kb.op("pool", lambda e: e.tensor_tensor(out=dst[:, t0:t0 + n], in0=t1[:, :n], in1=t2[:, :n],
                                                    op=ALU.add), [t1, t2], [dst])

        for (t0, n, lat) in self.tiles:
            ps = ps_a[rot % 3]
            rot += 1
            if lat:
                kb.mm(ps[:64, :n], rP[:], kpe[:, t0:t0 + n], True, True, [rP, kpe], [ps])
            apply_rope(krope, kpe[:, t0:t0 + n], [kpe], ps, t0, n, lat)
        wkv = ph.sb([128, 4, 4096], BF16)
        kb.dma("pool", wkv[:], wkvb[:, :, :], [wkvb_t], [wkv], sb=wkv)
        wkv_h = wkv.t[:, :, :].rearrange("p k (h x) -> p k h x", x=256)
        Kn = ph.sb([128, 4, T], BF16)
        V = ph.sb([128, NKC, 512], BF16)
        wq = ph.sb([128, 8, 192], BF16)
        wrot = ph.sb([128, 8, 64], BF16)
        qn = ph.sb([128, T], BF16)
        qr = ph.sb([64, T], BF16)
        pex = [ph.sb([128, 512], BF16) for _ in range(3)]
        rl = ph.sb([128, 512], F32)
        ot = [ph.sb([128, 512], BF16) for _ in range(2)]
        ev = 0
        for hg in range(4):
            for hl in range(4):
                h = hg * 4 + hl
                for (t0, n, lat) in self.tiles:
                    ps = ps_a[rot % 3]
                    rot += 1
                    for kc in range(4):
                        kb.mm(ps[:, :n], wkv[:, kc, h * 256:h * 256 + 128], ckvn[:, kc, t0:t0 + n],
                              kc == 0, kc == 3, [wkv, ckvn], [ps])
                    _evac(kb, ev, Kn[:, hl, t0:t0 + n], ps[:, :n], [ps], [Kn])
                    ev += 1
            for c in range(NKC):
                ps = ps_a[rot % 3]
                rot += 1
                for kc in range(4):
                    kb.mm(ps[:, :].rearrange("p (h x) -> p h x", x=128), ckvn[:, kc, c * 128:(c + 1) * 128],
                          wkv_h[:, kc, hg * 4:hg * 4 + 4, 128:256], kc == 0, kc == 3, [wkv, ckvn], [ps])
                _evac(kb, ev, V[:, c, :], ps[:, :], [ps], [V])
                ev += 1
            for hl in range(4):
                h = hg * 4 + hl
                kb.dma("pool", wq[:], wqb[:, :, h * 192:(h + 1) * 192], [wqb_t], [wq], sb=wq)
                for (a, b_, sg) in ((0, 144, -1.0), (16, 128, 1.0), (32, 176, -1.0), (48, 160, 1.0)):
                    kb.op("act", lambda e: e.mul(out=wrot[:, :, a:a + 16], in_=wq[:, :, b_:b_ + 16], mul=sg),
                          [wq], [wrot])
                for (t0, n, lat) in self.tiles:
                    if last and not lat:
                        continue
                    ps = ps_a[rot % 3]
                    rot += 1
                    for kc in range(8):
                        kb.mm(ps[:, :n], wq[:, kc, 0:128], cqn[:, kc, t0:t0 + n], kc == 0, kc == 7, [wq, cqn], [ps])
                    _evac(kb, ev, qn[:, t0:t0 + n], ps[:, :n], [ps], [qn])
                    ev += 1
                    ps1 = ps_a[rot % 3]
                    rot += 1
                    for kc in range(8):
                        kb.mm(ps1[:64, :n], wq[:, kc, 128:192], cqn[:, kc, t0:t0 + n], kc == 0, kc == 7,
                              [wq, cqn], [ps1])
                    ps2 = ps_a[rot % 3]
                    rot += 1
                    if lat:
                        for kc in range(8):
                            kb.mm(ps2[:64, :n], wrot[:, kc, :], cqn[:, kc, t0:t0 + n], kc == 0, kc == 7,
                                  [wrot, cqn], [ps2])
                    apply_rope(qr, ps1[:64, :n], [ps1], ps2, t0, n, lat)
                for qi, (t0, n, lat) in enumerate(self.tiles):
                    if last and not lat:
                        continue
                    nkeys = NKC if lat else TC // 128
                    po, pl = ps_o[qi % 2], ps_l[qi % 2]
                    for c in range(nkeys):
                        ps = ps_a[rot % 3]
                        px = pex[rot % 3]
                        rot += 1
                        kb.mm(ps[:, :n], Kn[:, hl, c * 128:(c + 1) * 128], qn[:, t0:t0 + n], True, False,
                              [Kn, qn], [ps])
                        kb.mm(ps[:, :n], krope[:, c * 128:(c + 1) * 128], qr[:, t0:t0 + n], False, True,
                              [krope, qr], [ps])
                        kb.op("act", lambda e: e.activation(out=px[:, :n], in_=ps[:, :n], func=AF.Exp, scale=scale),
                              [ps], [px])
                        kb.mm(po[:, :n], V[:, c, hl * 128:(hl + 1) * 128], px[:, :n], c == 0, c == nkeys - 1,
                              [V, px], [po])
                        kb.mm(pl[:, :n], ones_bf[:], px[:, :n], c == 0, c == nkeys - 1, [ones_bf, px], [pl])
                    kb.op("dve", lambda e: e.reciprocal(out=rl[:, :n], in_=pl[:, :n]), [pl], [rl])
                    o_ = ot[qi % 2]
                    kb.op("dve", lambda e: e.tensor_tensor(out=o_[:, :n], in0=po[:, :n], in1=rl[:, :n], op=ALU.mult),
                          [po, rl], [o_])
                    kb.dma("sp", yaT[h * 128:(h + 1) * 128, t0:t0 + n], o_[:, :n], [o_], [yaT], sb=o_)


Model.phase_mla = phase_mla


def rope_tables(TL):
    rows = TL // GRID_W
    row = np.repeat(np.arange(rows, dtype=np.float32), GRID_W)
    colp = np.tile(np.arange(GRID_W, dtype=np.float32), rows)
    half = A_ROPE // 2
    freq = (ROPE_THETA ** (-np.arange(0, half, 2, dtype=np.float32) / half)).astype(np.float32)
    ar = row[None, :] * freq[:, None]
    ac = colp[None, :] * freq[:, None]
    C = np.concatenate([np.cos(ar), np.cos(ar), np.cos(ac), np.cos(ac)], 0).astype(np.float32)
    S = np.concatenate([np.sin(ar), np.sin(ar), np.sin(ac), np.sin(ac)], 0).astype(np.float32)
    P = np.zeros((64, 64), np.float32)
    for m in range(16):
        P[m, 16 + m] = -1.0
        P[16 + m, m] = 1.0
        P[32 + m, 48 + m] = -1.0
        P[48 + m, 32 + m] = 1.0
    return C, S, np.ascontiguousarray(P.T)


def _groups(tiles, mx):
    groups, cur, tot = [], [], 0
    for tl in tiles:
        if cur and tot + tl[1] > mx:
            groups.append(cur)
            cur, tot = [], 0
        cur.append(tl)
        tot += tl[1]
    if cur:
        groups.append(cur)
    return groups


def phase_merge(self, l, last):
    kb = self.kb
    self.load_vecs()
    T = self.T
    xs = self.dscr("xs", [D, T])
    pT = self.dscr("pT", [IN_COLS, T])
    srcs = [(self.dscr("yaT", [A_WIDTH, T], BF16), 16, self.din("w_up_a", [DEPTH, A_WIDTH, D])),
            (self.dscr("ybT", [B_WIDTH, T], BF16), 8, self.din("w_up_b", [DEPTH, B_WIDTH, D])),
            (self.dscr("ycT", [C_WIDTH, T], BF16), 8, self.din("w_up_c", [DEPTH, C_WIDTH, D]))]
    wout_t = self.din("w_out", [DEPTH, D, D])
    wout = wout_t.t[l].rearrange("(kc p) c -> p kc c", p=128)
    mod = self.mod[l]
    tiles = [t for t in self.tiles if t[2] or not last]
    with Phase(kb) as ph:
        ys = [ph.sb([128, nk, 1024], BF16) for (_, nk, _) in srcs]
        mixed = ph.sb([128, 32, 1024], BF16)
        wts = [[ph.sb([128, nk, 128], BF16) for _ in range(2)] for (_, nk, _) in srcs]
        wo = [ph.sb([128, 32, 128], BF16) for _ in range(2)]
        gt = [ph.sb([128, 512], F32) for _ in range(3)]
        acc = [ph.sb([128, 512], F32) for _ in range(2)]
        xt = [ph.sb([128, 512], F32) for _ in range(2)]
        pss = [ph.ps([128, 512]) for _ in range(6)]
        rot = 0
        for grp in _groups(tiles, 1024):
            off = 0
            for (t0, n, lat) in grp:
                for (y_d, nk, _), y_s in zip(srcs, ys):
                    kb.dma("sp", y_s[:, :, off:off + n],
                           y_d.t.rearrange("(kc p) t -> p kc t", p=128)[:, :, t0:t0 + n], [y_d], [y_s], sb=y_s)
                off += n
            for m in range(32):
                for (y_d, nk, w_d), wpair in zip(srcs, wts):
                    wt = wpair[m % 2]
                    kb.dma("pool", wt[:], w_d.t[l].rearrange("(kc p) c -> p kc c", p=128)[:, :, m * 128:(m + 1) * 128],
                           [w_d], [wt], sb=wt)
                off = 0
                for (t0, n, lat) in grp:
                    a_ = acc[rot % 2]
                    for bi, ((y_d, nk, w_d), y_s, wpair) in enumerate(zip(srcs, ys, wts)):
                        wt = wpair[m % 2]
                        ps = pss[rot % 6]
                        rot += 1
                        for kc in range(nk):
                            kb.mm(ps[:, :n], wt[:, kc, :], y_s[:, kc, off:off + n], kc == 0, kc == nk - 1,
                                  [wt, y_s], [ps])
                        g_ = gt[bi]
                        r0 = OFF_G + bi * D + m * 128
                        kb.dma("sp", g_[:, :n], pT[r0:r0 + 128, t0:t0 + n], [pT], [g_], sb=g_)
                        kb.op("act", lambda e: e.activation(out=g_[:, :n], in_=g_[:, :n], func=AF.Sigmoid), [g_], [g_])
                        if bi == 0:
                            kb.op("dve", lambda e: e.tensor_tensor(out=a_[:, :n], in0=ps[:, :n], in1=g_[:, :n],
                                                                   op=ALU.mult), [ps, g_], [a_])
                        else:
                            kb.op("dve", lambda e: e.tensor_tensor(out=g_[:, :n], in0=ps[:, :n], in1=g_[:, :n],
                                                                   op=ALU.mult), [ps, g_], [g_])
                            if bi == 1:
                                kb.op("pool", lambda e: e.tensor_tensor(out=a_[:, :n], in0=a_[:, :n], in1=g_[:, :n],
                                                                        op=ALU.add), [a_, g_], [a_])
                            else:
                                kb.op("pool", lambda e: e.tensor_tensor(out=mixed[:, m, off:off + n], in0=a_[:, :n],
                                                                        in1=g_[:, :n], op=ALU.add), [a_, g_], [mixed])
                    off += n
            for m in range(32):
                w_ = wo[m % 2]
                kb.dma("pool", w_[:], wout[:, :, m * 128:(m + 1) * 128], [wout_t], [w_], sb=w_)
                off = 0
                for (t0, n, lat) in grp:
                    r = 0 if lat else 1
                    ps = pss[rot % 6]
                    x_ = xt[rot % 2]
                    rot += 1
                    for kc in range(32):
                        kb.mm(ps[:, :n], w_[:, kc, :], mixed[:, kc, off:off + n], kc == 0, kc == 31, [w_, mixed], [ps])
                    kb.dma("sp", x_[:, :n], xs[m * 128:(m + 1) * 128, t0:t0 + n], [xs], [x_], sb=x_)
                    kb.op("dve", lambda e: e.scalar_tensor_tensor(
                        out=x_[:, :n], in0=ps[:, :n], scalar=mod[:, 64 + m, r:r + 1], in1=x_[:, :n],
                        op0=ALU.mult, op1=ALU.add), [ps, mod, x_], [x_])
                    kb.dma("sp", xs[m * 128:(m + 1) * 128, t0:t0 + n], x_[:, :n], [x_], [xs], sb=x_)
                    off += n


Model.phase_merge = phase_merge


def phase_final(self):
    kb = self.kb
    T, TC, TL = self.T, self.TC, self.TL
    xs = self.dscr("xs", [D, T])
    fg_d = self.din("fng", [128, 32])
    yo = self.dout("outT", [D, TL])
    xsv = xs.t.rearrange("(kc p) t -> p kc t", p=128)
    yov = yo.t.rearrange("(kc p) t -> p kc t", p=128)
    with Phase(kb) as ph:
        self.consts(ph)
        fg = ph.sb([128, 32], F32)
        kb.dma("sp", fg[:], fg_d[:], [fg_d], [fg], sb=fg)
        xt = [ph.sb([128, 32, 256], F32) for _ in range(2)]
        sq = [ph.sb([128, 512], F32) for _ in range(2)]
        rstd = ph.sb([128, 512], F32)
        ps_ss = ph.ps([128, 512])
        i = 0
        for (t0, n, lat) in _subtiles(self.tiles, 256):
            if not lat:
                continue
            x_ = xt[i % 2]
            i += 1
            kb.dma("sp", x_[:, :, :n], xsv[:, :, t0:t0 + n], [xs], [x_], sb=x_)
            self.rms_stats(ph, lambda kc: x_[:, kc, :n], 32, n, D, self.ones, sq, ps_ss, rstd, [x_])
            for kc in range(32):
                kb.op("dve", lambda e: e.scalar_tensor_tensor(
                    out=x_[:, kc, :n], in0=x_[:, kc, :n], scalar=fg[:, kc:kc + 1], in1=rstd[:, :n],
                    op0=ALU.mult, op1=ALU.mult), [x_, fg, rstd], [x_])
            kb.dma("sp", yov[:, :, t0 - TC:t0 - TC + n], x_[:, :, :n], [x_], [yo], sb=x_)


Model.phase_final = phase_final


def phase_copy_in(self):
    kb = self.kb
    T = self.T
    x0 = self.din("x0", [D, T])
    xs = self.dscr("xs", [D, T])
    with Phase(kb) as ph:
        ts = [ph.sb([128, T], F32) for _ in range(2)]
        for i in range(32):
            t = ts[i % 2]
            kb.dma("sp", t[:], x0[i * 128:(i + 1) * 128, :], [x0], [t], sb=t)
            kb.dma("sp", xs[i * 128:(i + 1) * 128, :], t[:], [t], [xs], sb=t)


Model.phase_copy_in = phase_copy_in

TC_FULL, TL_FULL, BATCH = 256, 2048, 4


def host_consts():
    ident = np.eye(128, dtype=np.float32)
    selA = np.zeros((128, 128, 128), np.float32)
    for s in range(128):
        selA[s, s, :] = 1.0
    selR = np.zeros((128, 64, 128), np.float32)
    for s in range(64):
        selR[s, s, :64] = 1.0
        selR[127 - s, s, 64:] = 1.0
    return ident, selA, selR


def anti_identity():
    return np.ascontiguousarray(np.eye(128, dtype=np.float32)[::-1])


def _time_reverse_block(kb, src_ap, src_reads, ident, antiI, ps_a, ps_b, sb_a, dst_ap, dst_writes):
    kb.op("pe", lambda e: e.transpose(ps_a[:, 0:128], src_ap, ident[:]), src_reads + [ident], [ps_a])
    kb.op("act", lambda e: e.copy(out=sb_a[:, 0:128], in_=ps_a[:, 0:128]), [ps_a], [sb_a])
    kb.mm(ps_b[:, 0:128], sb_a[:, 0:128], antiI[:], True, True, [sb_a, antiI], [ps_b])
    kb.op("dve", lambda e: e.tensor_copy(out=dst_ap, in_=ps_b[:, 0:128]), [ps_b], dst_writes)


def _transpose_out(self, ph, kb, srcs, ident, ps_t, tt, dst, t0, n, c, rot0):
    rot = rot0
    for j0 in range(0, n, 128):
        p = ps_t[rot % len(ps_t)]
        o = tt[rot % len(tt)]
        rot += 1
        for vi, (vec, src, reads) in enumerate(srcs):
            kb.op("pe", lambda e: e.transpose(p[:, vi * 128:(vi + 1) * 128], src[:, j0:j0 + 128], ident[:]),
                  reads + [ident], [p])
        nv = len(srcs)
        _evac(kb, rot, o[:, :nv * 128], p[:, :nv * 128], [p], [o])
        for vi, (vec, src, reads) in enumerate(srcs):
            kb.dma("sp", dst[t0 + j0:t0 + j0 + 128, vec, c * 128:(c + 1) * 128], o[:, vi * 128:(vi + 1) * 128],
                   [o], [dst], sb=o)
    return rot


def phase_rwkv_prep(self, l):
    kb = self.kb
    self.load_vecs()
    T, TC, TL = self.T, self.TC, self.TL
    pT = self.dscr("pT", [IN_COLS, T])
    X = self.dscr("rkvX", [T, 8, 1024])
    vT = self.dscr("rkv_vT", [1024, T])
    vTr = self.dscr("rkv_vTr", [1024, T])
    anti_d = self.din("antiI", [128, 128])
    gT = self.dscr("rkv_gT", [1024, T])
    bT = self.dscr("rkv_bT", [1024, T])
    w2_t = self.din("rwkv_w2", [DEPTH, 2, 128, 1024])
    a2_t = self.din("rwkv_a2", [DEPTH, 2, 128, 1024])
    g2_t = self.din("rwkv_g2", [DEPTH, 256, 1024])
    ident_d = self.din("ident", [128, 128])
    segs = [(0, TC), (TC, T)]
    mu = self.vec(l, "mu")
    with Phase(kb) as ph:
        self.consts(ph)
        ident = ph.sb([128, 128], F32)
        kb.dma("sp", ident[:], ident_d[:], [ident_d], [ident], sb=ident)
        antiI = ph.sb([128, 128], F32)
        kb.dma("sp", antiI[:], anti_d[:], [anti_d], [antiI], sb=antiI)
        vrev = ph.sb([128, T], F32)
        rv_a = ph.sb([128, 128], F32)
        bones = ph.sb([128, 128], F32)
        kb.op("pool", lambda e: e.memset(bones[:], 0.0), [], [bones])
        kb.op("pool", lambda e: e.memset(bones[0:64, 0:64], 1.0), [], [bones])
        kb.op("pool", lambda e: e.memset(bones[64:128, 64:128], 1.0), [], [bones])
        w2 = ph.sb([128, 2, 1024], F32)
        a2 = ph.sb([128, 2, 1024], F32)
        g2 = ph.sb([128, 2, 1024], F32)
        kb.dma("sp", w2[:], w2_t.t[l].rearrange("d r c -> r d c"), [w2_t], [w2], sb=w2)
        kb.dma("sp", a2[:], a2_t.t[l].rearrange("d r c -> r d c"), [a2_t], [a2], sb=a2)
        kb.dma("sp", g2[:], g2_t.t[l].rearrange("(k r) c -> r k c", r=128), [g2_t], [g2], sb=g2)
        m1 = ph.sb([128, 28], F32)
        m2 = ph.sb([128, 28], F32)
        kb.op("dve", lambda e: e.tensor_scalar(out=m1[:], in0=mu, scalar1=-1.0, scalar2=1.0, op0=ALU.mult,
                                               op1=ALU.add), [self.vecs], [m1])
        kb.op("dve", lambda e: e.tensor_scalar(out=m2[:], in0=mu, scalar1=0.5, scalar2=None, op0=ALU.mult),
              [self.vecs], [m2])
        omk = ph.sb([128, 8], F32)
        kb.op("dve", lambda e: e.tensor_scalar(out=omk[:], in0=self.vec(l, "k_a"), scalar1=-1.0, scalar2=1.0,
                                               op0=ALU.mult, op1=ALU.add), [self.vecs], [omk])
        xin = [ph.sb([128, T], F32) for _ in range(2)]
        nb = ph.sb([128, T], F32)

        def load_shift(ci, dst):
            x_ = xin[ci % 2]
            kb.dma("sp", x_[:], pT[OFF_B + ci * 128:OFF_B + (ci + 1) * 128, :], [pT], [x_], sb=x_)
            for (s0, s1) in segs:
                kb.op("pool", lambda e: e.tensor_tensor(out=nb[:, s0 + 1:s1 - 1], in0=x_[:, s0:s1 - 2],
                                                        in1=x_[:, s0 + 2:s1], op=ALU.add), [x_], [nb])
                kb.op("pool", lambda e: e.tensor_copy(out=nb[:, s0:s0 + 1], in_=x_[:, s0 + 1:s0 + 2]), [x_], [nb])
                kb.op("pool", lambda e: e.tensor_copy(out=nb[:, s1 - 1:s1], in_=x_[:, s1 - 2:s1 - 1]), [x_], [nb])
            kb.op("dve", lambda e: e.tensor_scalar(out=nb[:], in0=nb[:], scalar1=m2[:, ci:ci + 1], scalar2=None,
                                                   op0=ALU.mult), [nb, m2], [nb])
            kb.op("dve", lambda e: e.scalar_tensor_tensor(out=dst, in0=x_[:], scalar=m1[:, ci:ci + 1], in1=nb[:],
                                                          op0=ALU.mult, op1=ALU.add), [x_, m1, nb], [dst_t[0]])

        dst_t = [None]
        tw = ph.sb([128, T], F32)
        al = ph.sb([128, T], F32)
        sg = ph.sb([128, 2, T], F32)
        dst_t[0] = tw
        load_shift(24, tw[:])
        kb.op("act", lambda e: e.activation(out=tw[:], in_=tw[:], func=AF.Tanh), [tw], [tw])
        dst_t[0] = al
        load_shift(25, al[:])
        dst_t[0] = sg
        load_shift(26, sg[:, 0, :])
        load_shift(27, sg[:, 1, :])
        kb.op("act", lambda e: e.activation(out=sg[:], in_=sg[:], func=AF.Sigmoid), [sg], [sg])
        rr = ph.sb([128, T], F32)
        kk_ = ph.sb([128, T], F32)
        vv = ph.sb([128, T], F32)
        d = {k_: [ph.sb([128, 512], F32) for _ in range(2)] for k_ in ("w", "a", "kd", "ka")}
        t_kk = ph.sb([128, 512], F32)
        t_a = ph.sb([128, 512], F32)
        t_b = ph.sb([128, 512], F32)
        t_g = ph.sb([128, 512], F32)
        ps_m = [ph.ps([128, 512]) for _ in range(3)]
        ps_t = [ph.ps([128, 1024]) for _ in range(2)]
        tt = [ph.sb([128, 1024], F32) for _ in range(2)]
        k_k, k_a, r_k = self.vec(l, "k_k"), self.vec(l, "k_a"), self.vec(l, "r_k")
        w0, a0 = self.vec(l, "w0"), self.vec(l, "a0")
        rot = 0
        trot = 0
        c_decay = -math.exp(-0.5)
        for c in range(8):
            dst_t[0] = rr
            load_shift(c, rr[:])
            dst_t[0] = kk_
            load_shift(8 + c, kk_[:])
            dst_t[0] = vv
            load_shift(16 + c, vv[:])
            kb.dma("sp", vT[c * 128:(c + 1) * 128, :], vv[:], [vv], [vT], sb=vv)
            for (s0, s1) in segs:
                for k in range((s1 - s0) // 128):
                    src0 = s0 + 128 * k
                    dst0 = s1 - 128 * (k + 1)
                    _time_reverse_block(kb, vv[:, src0:src0 + 128], [vv], ident, antiI, ps_m[0], ps_m[1], rv_a,
                                        vrev[:, dst0:dst0 + 128], [vrev])
            kb.dma("sp", vTr[c * 128:(c + 1) * 128, :], vrev[:], [vrev], [vTr], sb=vrev)
            for (t0, n, lat) in self.tiles:
                sl = slice(t0, t0 + n)
                for dd in range(2):
                    p = ps_m[rot % 3]
                    rot += 1
                    kb.mm(p[:, :n], w2[:, dd, c * 128:(c + 1) * 128], tw[:, sl], True, True, [w2, tw], [p])
                    wd = d["w"][dd]
                    kb.op("act", lambda e: e.activation(out=wd[:, :n], in_=p[:, :n], func=AF.Sigmoid,
                                                        bias=w0[:, dd * 8 + c:dd * 8 + c + 1], scale=1.0),
                          [p, self.vecs], [wd])
                    kb.op("act", lambda e: e.activation(out=wd[:, :n], in_=wd[:, :n], func=AF.Exp, scale=c_decay),
                          [wd], [wd])
                    p = ps_m[rot % 3]
                    rot += 1
                    kb.mm(p[:, :n], a2[:, dd, c * 128:(c + 1) * 128], al[:, sl], True, True, [a2, al], [p])
                    ad = d["a"][dd]
                    kb.op("act", lambda e: e.activation(out=ad[:, :n], in_=p[:, :n], func=AF.Sigmoid,
                                                        bias=a0[:, dd * 8 + c:dd * 8 + c + 1], scale=1.0),
                          [p, self.vecs], [ad])
                kb.op("dve", lambda e: e.tensor_scalar(out=t_kk[:, :n], in0=kk_[:, sl], scalar1=k_k[:, c:c + 1],
                                                       scalar2=None, op0=ALU.mult), [kk_, self.vecs], [t_kk])
                kb.op("act", lambda e: e.activation(out=t_a[:, :n], in_=t_kk[:, :n], func=AF.Square), [t_kk], [t_a])
                p = ps_m[rot % 3]
                rot += 1
                kb.mm(p[:, :n], bones[:], t_a[:, :n], True, True, [bones, t_a], [p])
                kb.op("act", lambda e: e.activation(out=t_a[:, :n], in_=p[:, :n], func=AF.Sqrt,
                                                    bias=self.eps_t[:, 0:1], scale=1.0), [p, self.eps_t], [t_a])
                kb.op("dve", lambda e: e.reciprocal(out=t_a[:, :n], in_=t_a[:, :n]), [t_a], [t_a])
                kb.op("dve", lambda e: e.tensor_tensor(out=t_kk[:, :n], in0=t_kk[:, :n], in1=t_a[:, :n], op=ALU.mult),
                      [t_kk, t_a], [t_kk])
                for dd in range(2):
                    ad, kd, ka = d["a"][dd], d["kd"][dd], d["ka"][dd]
                    kb.op("dve", lambda e: e.tensor_scalar(out=kd[:, :n], in0=ad[:, :n], scalar1=k_a[:, c:c + 1],
                                                           scalar2=omk[:, c:c + 1], op0=ALU.mult, op1=ALU.add),
                          [ad, self.vecs, omk], [kd])
                    kb.op("dve", lambda e: e.tensor_tensor(out=kd[:, :n], in0=kd[:, :n], in1=kk_[:, sl], op=ALU.mult),
                          [kd, kk_], [kd])
                    kb.op("pool", lambda e: e.tensor_tensor(out=ka[:, :n], in0=t_kk[:, :n], in1=ad[:, :n],
                                                            op=ALU.mult), [t_kk, ad], [ka])
                kb.op("pool", lambda e: e.tensor_tensor(out=t_b[:, :n], in0=d["kd"][0][:, :n], in1=d["kd"][1][:, :n],
                                                        op=ALU.add), [d["kd"][0], d["kd"][1]], [t_b])
                kb.op("dve", lambda e: e.scalar_tensor_tensor(out=t_b[:, :n], in0=rr[:, sl], scalar=r_k[:, c:c + 1],
                                                              in1=t_b[:, :n], op0=ALU.mult, op1=ALU.mult),
                      [rr, self.vecs, t_b], [t_b])
                p = ps_m[rot % 3]
                rot += 1
                kb.mm(p[:, :n], bones[:], t_b[:, :n], True, True, [bones, t_b], [p])
                kb.op("dve", lambda e: e.tensor_tensor(out=t_b[:, :n], in0=p[:, :n], in1=vv[:, sl], op=ALU.mult),
                      [p, vv], [t_b])
                kb.dma("sp", bT[c * 128:(c + 1) * 128, sl], t_b[:, :n], [t_b], [bT], sb=t_b)
                p = ps_m[rot % 3]
                rot += 1
                for k2 in range(2):
                    kb.mm(p[:, :n], g2[:, k2, c * 128:(c + 1) * 128], sg[:, k2, sl], k2 == 0, k2 == 1, [g2, sg], [p])
                kb.op("act", lambda e: e.copy(out=t_g[:, :n], in_=p[:, :n]), [p], [t_g])
                kb.dma("sp", gT[c * 128:(c + 1) * 128, sl], t_g[:, :n], [t_g], [gT], sb=t_g)
                srcs = [(0, t_kk, [t_kk]), (1, rr[:, sl], [rr]), (2, d["w"][0], [d["w"][0]]), (3, d["w"][1], [d["w"][1]]),
                        (4, d["ka"][0], [d["ka"][0]]), (5, d["ka"][1], [d["ka"][1]]),
                        (6, d["kd"][0], [d["kd"][0]]), (7, d["kd"][1], [d["kd"][1]])]
                trot = _transpose_out(self, ph, kb, srcs, ident, ps_t, tt, X, t0, n, c, trot)


Model.phase_rwkv_prep = phase_rwkv_prep


def phase_scan(self, mode, xname, vname, yname):
    kb = self.kb
    T, TC = self.T, self.TC
    X = self.dscr(xname, [T, 8, 1024])
    vT = self.dscr(vname, [1024, T])
    yF = self.dscr(yname + "F", [1024, T])
    yB = self.dscr(yname + "B", [1024, T])
    rw = mode == "rwkv"
    G, J = (16, 64) if rw else (16, 128)
    GH = G if rw else 8
    FW = G * J
    sel_d = self.din("selR", [128, 64, 128]) if rw else self.din("selA", [128, 128, 128])
    vview = vT.t.rearrange("(h i) t -> i h t", i=(64 if rw else 128))
    if rw:
        vTr = self.dscr(vname + "r", [1024, T])
        vrview = vTr.t.rearrange("(h i) t -> i h t", i=64)
    yFv = yF.t.rearrange("(h i) t -> i h t", i=(64 if rw else 128))
    yBv = yB.t.rearrange("(h i) t -> i h t", i=(64 if rw else 128))
    vecF = [0, 2, 4, 6, 1]
    vecB = [0, 3, 5, 7, 1]
    with Phase(kb) as ph:
        sel = ph.sb([128, 64 if rw else 128, 128], F32)
        kb.dma("sp", sel[:], sel_d[:], [sel_d], [sel], sb=sel)
        nU = 1
        S = [ph.sb([128, G, J], F32) for _ in range(nU)]
        for s_ in S:
            kb.op("pool", lambda e: e.memset(s_[:], 0.0), [], [s_])
        tmp = [ph.sb([128, G, J], F32) for _ in range(2)]
        sa = ph.sb([128, G], F32)
        Xb = [[ph.sb([128, 1024], F32) for _ in range(5)] for _ in range(2)]
        if rw:
            Vb = [[ph.sb([128, G, 64], F32)] for _ in range(2)]
            Yb = [[ph.sb([128, G, 64], F32)] for _ in range(2)]
            bc = [ph.ps([128, 1024]) for _ in range(4)]
        else:
            Vb = [[ph.sb([128, GH, 64], F32) for _ in range(2)] for _ in range(2)]
            Yb = [[ph.sb([128, GH, 64], F32) for _ in range(2)] for _ in range(2)]
            bc = [ph.ps([128, 2048]) for _ in range(2)]
        rot = [0]

        def bcast(lhsT, xb):
            p = bc[rot[0] % len(bc)]
            rot[0] += 1
            if rw:
                kb.mm(p[:, 0:512], lhsT, xb[:, 0:512], True, True, [sel, xb], [p])
                kb.mm(p[:, 512:1024], lhsT, xb[:, 512:1024], True, True, [sel, xb], [p])
            else:
                lf, lb = lhsT
                kb.mm(p[:, 0:512], lf, xb[:, 0:512], True, True, [sel, xb], [p])
                kb.mm(p[:, 512:1024], lf, xb[:, 512:1024], True, True, [sel, xb], [p])
                kb.mm(p[:, 1024:1536], lb, xb[:, 0:512], True, True, [sel, xb], [p])
                kb.mm(p[:, 1536:2048], lb, xb[:, 512:1024], True, True, [sel, xb], [p])
            return p

        def v3(t):
            return t[:, :].rearrange("p (g j) -> p g j", j=J)

        bi_glob = 0
        for (seg0, L) in ((0, TC), (TC, T - TC)):
            for bi in range(L // 64):
                buf = bi_glob % 2
                bi_glob += 1
                tf0 = seg0 + 64 * bi
                tb0 = seg0 + L - 64 * (bi + 1)
                for j in range(5):
                    xb = Xb[buf][j]
                    kb.dma("sp", xb[0:64, :], X[tf0:tf0 + 64, vecF[j], :], [X], [xb], sb=xb)
                    kb.dma("sp", xb[64:128, :], X[tb0:tb0 + 64, vecB[j], :], [X], [xb], sb=xb)
                if rw:
                    vb = Vb[buf][0]
                    kb.dma("sp", vb[0:64], vview[:, :, tf0:tf0 + 64], [vT], [vb], sb=vb)
                    kb.dma("sp", vb[64:128], vrview[:, :, tf0:tf0 + 64], [vTr], [vb], sb=vb)
                else:
                    kb.dma("sp", Vb[buf][0][:], vview[:, :, tf0:tf0 + 64], [vT], [Vb[buf][0]], sb=Vb[buf][0])
                    kb.dma("sp", Vb[buf][1][:], vview[:, :, tb0:tb0 + 64], [vT], [Vb[buf][1]], sb=Vb[buf][1])
                for s in range(64):
                    for u in range(nU):
                        St = S[u]
                        if rw:
                            lhsT = sel[:, s, :]
                        else:
                            lhsT = (sel[:, s, :], sel[:, 127 - s, :])
                        t0_, t1_ = tmp
                        xs_ = Xb[buf]
                        pk = bcast(lhsT, xs_[0])
                        kb.op("dve", lambda e: e.tensor_tensor(out=t0_[:], in0=St[:], in1=v3(pk), op=ALU.mult),
                              [St, pk], [t0_])
                        kb.op("dve", lambda e: e.tensor_reduce(out=sa[:], in_=t0_[:], axis=AX.X, op=ALU.add,
                                                               negate=True), [t0_], [sa])
                        pw = bcast(lhsT, xs_[1])
                        kb.op("dve", lambda e: e.tensor_tensor(out=St[:], in0=St[:], in1=v3(pw), op=ALU.mult),
                              [St, pw], [St])
                        pa = bcast(lhsT, xs_[2])
                        kb.op("dve", lambda e: e.tensor_tensor(out=t1_[:], in0=v3(pa),
                                                               in1=sa[:].unsqueeze(2).to_broadcast([128, G, J]),
                                                               op=ALU.mult), [pa, sa], [t1_])
                        kb.op("dve", lambda e: e.tensor_tensor(out=St[:], in0=St[:], in1=t1_[:], op=ALU.add),
                              [St, t1_], [St])
                        pd = bcast(lhsT, xs_[3])
                        if rw:
                            vb = Vb[buf][0]
                            kb.op("dve", lambda e: e.tensor_tensor(
                                out=t0_[:], in0=v3(pd), in1=vb[:, :, s:s + 1].to_broadcast([128, G, J]),
                                op=ALU.mult), [pd, vb], [t0_])
                        else:
                            for (g0, vb, col) in ((0, Vb[buf][0], s), (8, Vb[buf][1], 63 - s)):
                                kb.op("dve", lambda e: e.tensor_tensor(
                                    out=t0_[:, g0:g0 + 8, :], in0=v3(pd)[:, g0:g0 + 8, :],
                                    in1=vb[:, :, col:col + 1].to_broadcast([128, 8, J]), op=ALU.mult),
                                    [pd, vb], [t0_])
                        kb.op("dve", lambda e: e.tensor_tensor(out=St[:], in0=St[:], in1=t0_[:], op=ALU.add),
                              [St, t0_], [St])
                        pr = bcast(lhsT, xs_[4])
                        kb.op("dve", lambda e: e.tensor_tensor(out=t1_[:], in0=St[:], in1=v3(pr), op=ALU.mult),
                              [St, pr], [t1_])
                        if rw:
                            yb = Yb[buf][0]
                            kb.op("dve", lambda e: e.tensor_reduce(out=yb[:, :, s], in_=t1_[:], axis=AX.X,
                                                                   op=ALU.add), [t1_], [yb])
                        else:
                            for (g0, yb, col) in ((0, Yb[buf][0], s), (8, Yb[buf][1], 63 - s)):
                                kb.op("dve", lambda e: e.tensor_reduce(out=yb[:, :, col], in_=t1_[:, g0:g0 + 8, :],
                                                                       axis=AX.X, op=ALU.add), [t1_], [yb])
                if rw:
                    yb = Yb[buf][0]
                    kb.dma("sp", yFv[:, :, tf0:tf0 + 64], yb[0:64], [yb], [yF], sb=yb)
                    kb.dma("sp", yBv[:, :, tf0:tf0 + 64], yb[64:128], [yb], [yB], sb=yb)
                else:
                    kb.dma("sp", yFv[:, :, tf0:tf0 + 64], Yb[buf][0][:], [Yb[buf][0]], [yF], sb=Yb[buf][0])
                    kb.dma("sp", yBv[:, :, tb0:tb0 + 64], Yb[buf][1][:], [Yb[buf][1]], [yB], sb=Yb[buf][1])


Model.phase_scan = phase_scan


def phase_rwkv_post(self, l):
    kb = self.kb
    self.load_vecs()
    T = self.T
    yF = self.dscr("rkv_yF", [1024, T])
    yB = self.dscr("rkv_yB", [1024, T])
    gT = self.dscr("rkv_gT", [1024, T])
    bT = self.dscr("rkv_bT", [1024, T])
    ybT = self.dscr("ybT", [B_WIDTH, T], BF16)
    lg, lb = self.vec(l, "lnx_g"), self.vec(l, "lnx_b")
    ident_d = self.din("ident", [128, 128])
    anti_d = self.din("antiI", [128, 128])
    segs = [(0, self.TC), (self.TC, T)]
    with Phase(kb) as ph:
        self.consts(ph)
        ident = ph.sb([128, 128], F32)
        antiI = ph.sb([128, 128], F32)
        kb.dma("sp", ident[:], ident_d[:], [ident_d], [ident], sb=ident)
        kb.dma("sp", antiI[:], anti_d[:], [anti_d], [antiI], sb=antiI)
        yrev = ph.sb([128, T], F32)
        ynat = ph.sb([128, T], F32)
        rv_a = ph.sb([128, 128], F32)
        bmean = ph.sb([128, 128], F32)
        kb.op("pool", lambda e: e.memset(bmean[:], 0.0), [], [bmean])
        kb.op("pool", lambda e: e.memset(bmean[0:64, 0:64], 1.0 / 64), [], [bmean])
        kb.op("pool", lambda e: e.memset(bmean[64:128, 64:128], 1.0 / 64), [], [bmean])
        a_ = [ph.sb([128, 512], F32) for _ in range(2)]
        b_ = [ph.sb([128, 512], F32) for _ in range(2)]
        sq = ph.sb([128, 512], F32)
        g_ = [ph.sb([128, 512], F32) for _ in range(2)]
        bo = [ph.sb([128, 512], F32) for _ in range(2)]
        o_ = [ph.sb([128, 512], BF16) for _ in range(2)]
        ps = [ph.ps([128, 512]) for _ in range(4)]
        i = 0
        for c in range(8):
            rs = slice(c * 128, (c + 1) * 128)
            kb.dma("sp", yrev[:], yB[rs, :], [yB], [yrev], sb=yrev)
            for (s0, s1) in segs:
                for k in range((s1 - s0) // 128):
                    src0 = s0 + 128 * k
                    dst0 = s1 - 128 * (k + 1)
                    _time_reverse_block(kb, yrev[:, src0:src0 + 128], [yrev], ident, antiI, ps[2], ps[3], rv_a,
                                        ynat[:, dst0:dst0 + 128], [ynat])
            for (t0, n, lat) in self.tiles:
                sl = slice(t0, t0 + n)
                ya, yb_, gg, bb, oo = a_[i % 2], b_[i % 2], g_[i % 2], bo[i % 2], o_[i % 2]
                pm, pv = ps[0], ps[1]
                i += 1
                kb.dma("sp", ya[:, :n], yF[rs, sl], [yF], [ya], sb=ya)
                kb.op("pool", lambda e: e.tensor_copy(out=yb_[:, :n], in_=ynat[:, sl]), [ynat], [yb_])
                kb.dma("sp", gg[:, :n], gT[rs, sl], [gT], [gg], sb=gg)
                kb.dma("sp", bb[:, :n], bT[rs, sl], [bT], [bb], sb=bb)
                kb.op("dve", lambda e: e.tensor_tensor(out=ya[:, :n], in0=ya[:, :n], in1=yb_[:, :n], op=ALU.add),
                      [ya, yb_], [ya])
                kb.mm(pm[:, :n], bmean[:], ya[:, :n], True, True, [bmean, ya], [pm])
                kb.op("dve", lambda e: e.tensor_tensor(out=ya[:, :n], in0=ya[:, :n], in1=pm[:, :n], op=ALU.subtract),
                      [ya, pm], [ya])
                kb.op("act", lambda e: e.activation(out=sq[:, :n], in_=ya[:, :n], func=AF.Square), [ya], [sq])
                kb.mm(pv[:, :n], bmean[:], sq[:, :n], True, True, [bmean, sq], [pv])
                kb.op("act", lambda e: e.activation(out=yb_[:, :n], in_=pv[:, :n], func=AF.Sqrt,
                                                    bias=self.eps_t[:, 1:2], scale=1.0), [pv, self.eps_t], [yb_])
                kb.op("dve", lambda e: e.reciprocal(out=yb_[:, :n], in_=yb_[:, :n]), [yb_], [yb_])
                kb.op("dve", lambda e: e.scalar_tensor_tensor(out=ya[:, :n], in0=ya[:, :n], scalar=lg[:, c:c + 1],
                                                              in1=yb_[:, :n], op0=ALU.mult, op1=ALU.mult),
                      [ya, yb_, self.vecs], [ya])
                kb.op("dve", lambda e: e.scalar_tensor_tensor(out=ya[:, :n], in0=ya[:, :n], scalar=lb[:, c:c + 1],
                                                              in1=bb[:, :n], op0=ALU.add, op1=ALU.add),
                      [ya, bb, self.vecs], [ya])
                kb.op("pool", lambda e: e.tensor_tensor(out=oo[:, :n], in0=ya[:, :n], in1=gg[:, :n], op=ALU.mult),
                      [ya, gg], [oo])
                kb.dma("sp", ybT[rs, sl], oo[:, :n], [oo], [ybT], sb=oo)


Model.phase_rwkv_post = phase_rwkv_post


def phase_gdn_prep(self, l):
    kb = self.kb
    self.load_vecs()
    T, TC = self.T, self.TC
    pT = self.dscr("pT", [IN_COLS, T])
    X = self.dscr("gdnX", [T, 8, 1024])
    vT = self.dscr("gdn_vT", [1024, T])
    ident_d = self.din("ident", [128, 128])
    selA_d = self.din("selA", [128, 128, 128])
    segs = [(0, TC), (TC, T)]
    cw = self.vec(l, "conv")
    with Phase(kb) as ph:
        self.consts(ph)
        ident = ph.sb([128, 128], F32)
        kb.dma("sp", ident[:], ident_d[:], [ident_d], [ident], sb=ident)
        s16 = ph.sb([16, 16, 128], F32)
        kb.dma("sp", s16[:], selA_d[0:16, 0:16, :], [selA_d], [s16], sb=s16)
        al = ph.sb([16, T], F32)
        be = ph.sb([16, T], F32)
        ab = ph.sb([16, T], F32)
        kb.dma("sp", al[:], pT[OFF_C + 4096:OFF_C + 4112, :], [pT], [al], sb=al)
        kb.dma("sp", be[:], pT[OFF_C + 4112:OFF_C + 4128, :], [pT], [be], sb=be)
        nea = ph.sb([16, 1], F32)
        kb.op("act", lambda e: e.activation(out=nea[:], in_=self.vecs.t[0:16, l, VEC_OFF["alog"][0]:VEC_OFF["alog"][0] + 1],
                                            func=AF.Exp), [self.vecs], [nea])
        kb.op("dve", lambda e: e.tensor_scalar(out=nea[:], in0=nea[:], scalar1=-1.0, scalar2=None, op0=ALU.mult),
              [nea], [nea])
        dtb = self.vecs.t[0:16, l, VEC_OFF["dtb"][0]:VEC_OFF["dtb"][0] + 1]
        kb.op("act", lambda e: e.activation(out=al[:], in_=al[:], func=AF.Exp, bias=dtb, scale=1.0),
              [al, self.vecs], [al])
        kb.op("act", lambda e: e.activation(out=al[:], in_=al[:], func=AF.Ln, bias=self.one_t[0:16, 0:1], scale=1.0),
              [al, self.one_t], [al])
        kb.op("act", lambda e: e.activation(out=al[:], in_=al[:], func=AF.Exp, scale=nea[:, 0:1]), [al, nea], [al])
        kb.op("act", lambda e: e.activation(out=be[:], in_=be[:], func=AF.Sigmoid), [be], [be])
        kb.op("dve", lambda e: e.tensor_tensor(out=ab[:], in0=al[:], in1=be[:], op=ALU.mult), [al, be], [ab])
        xin = [ph.sb([128, T], F32) for _ in range(2)]
        qq = ph.sb([128, T], F32)
        kk_ = ph.sb([128, T], F32)
        vv = ph.sb([128, T], F32)
        t_a = ph.sb([128, 512], F32)
        wd = [ph.sb([128, 512], F32) for _ in range(2)]
        ka = [ph.sb([128, 512], F32) for _ in range(2)]
        kbt = [ph.sb([128, 512], F32) for _ in range(2)]
        ps_m = [ph.ps([128, 512]) for _ in range(3)]
        ps_t = [ph.ps([128, 1024]) for _ in range(2)]
        tt = [ph.sb([128, 1024], F32) for _ in range(2)]
        rot = 0
        trot = 0
        li = [0]

        def conv_silu(ci, dst):
            x_ = xin[li[0] % 2]
            li[0] += 1
            kb.dma("sp", x_[:], pT[OFF_C + ci * 128:OFF_C + (ci + 1) * 128, :], [pT], [x_], sb=x_)
            kb.op("dve", lambda e: e.tensor_scalar(out=dst[:], in0=x_[:], scalar1=cw[:, ci * 5 + 2:ci * 5 + 3],
                                                   scalar2=None, op0=ALU.mult), [x_, self.vecs], [dst])
            for (s0, s1) in segs:
                for tap in (0, 1, 3, 4):
                    off = tap - 2
                    a, b_ = max(s0, s0 - off), min(s1, s1 - off)
                    kb.op("dve", lambda e: e.scalar_tensor_tensor(
                        out=dst[:, a:b_], in0=x_[:, a + off:b_ + off], scalar=cw[:, ci * 5 + tap:ci * 5 + tap + 1],
                        in1=dst[:, a:b_], op0=ALU.mult, op1=ALU.add), [x_, self.vecs, dst], [dst])
            kb.op("act", lambda e: e.activation(out=dst[:], in_=dst[:], func=AF.Silu), [dst], [dst])

        def l2n(dst, scl):
            for (t0, n, lat) in self.tiles:
                sl = slice(t0, t0 + n)
                nonlocal rot
                p = ps_m[rot % 3]
                rot += 1
                kb.op("act", lambda e: e.activation(out=t_a[:, :n], in_=dst[:, sl], func=AF.Square), [dst], [t_a])
                kb.mm(p[:, :n], self.ones[:], t_a[:, :n], True, True, [self.ones, t_a], [p])
                kb.op("act", lambda e: e.activation(out=t_a[:, :n], in_=p[:, :n], func=AF.Sqrt,
                                                    bias=self.eps_t[:, 0:1], scale=1.0), [p, self.eps_t], [t_a])
                kb.op("dve", lambda e: e.reciprocal(out=t_a[:, :n], in_=t_a[:, :n]), [t_a], [t_a])
                kb.op("dve", lambda e: e.scalar_tensor_tensor(out=dst[:, sl], in0=dst[:, sl], scalar=scl,
                                                              in1=t_a[:, :n], op0=ALU.mult, op1=ALU.mult),
                      [dst, t_a], [dst])

        for h in range(8):
            conv_silu(h, qq)
            l2n(qq, C_HDIM ** -0.5)
            conv_silu(8 + h, kk_)
            l2n(kk_, 1.0)
            conv_silu(16 + h, vv)
            kb.dma("sp", vT[h * 128:(h + 1) * 128, :], vv[:], [vv], [vT], sb=vv)
            for (t0, n, lat) in self.tiles:
                sl = slice(t0, t0 + n)
                for dd in range(2):
                    r_ = dd * 8 + h
                    p = ps_m[rot % 3]
                    rot += 1
                    kb.mm(p[:, :n], s16[:, r_, :], al[:, sl], True, True, [s16, al], [p])
                    kb.op("act", lambda e: e.copy(out=wd[dd][:, :n], in_=p[:, :n]), [p], [wd[dd]])
                    p = ps_m[rot % 3]
                    rot += 1
                    kb.mm(p[:, :n], s16[:, r_, :], ab[:, sl], True, True, [s16, ab], [p])
                    kb.op("dve", lambda e: e.tensor_tensor(out=ka[dd][:, :n], in0=p[:, :n], in1=kk_[:, sl], op=ALU.mult),
                          [p, kk_], [ka[dd]])
                    p = ps_m[rot % 3]
                    rot += 1
                    kb.mm(p[:, :n], s16[:, r_, :], be[:, sl], True, True, [s16, be], [p])
                    kb.op("dve", lambda e: e.tensor_tensor(out=kbt[dd][:, :n], in0=p[:, :n], in1=kk_[:, sl],
                                                           op=ALU.mult), [p, kk_], [kbt[dd]])
                srcs = [(0, kk_[:, sl], [kk_]), (1, qq[:, sl], [qq]), (2, wd[0], [wd[0]]), (3, wd[1], [wd[1]]),
                        (4, ka[0], [ka[0]]), (5, ka[1], [ka[1]]), (6, kbt[0], [kbt[0]]), (7, kbt[1], [kbt[1]])]
                trot = _transpose_out(self, ph, kb, srcs, ident, ps_t, tt, X, t0, n, h, trot)


Model.phase_gdn_prep = phase_gdn_prep


def phase_gdn_post(self, l):
    kb = self.kb
    self.load_vecs()
    T = self.T
    yF = self.dscr("gdn_yF", [1024, T])
    yB = self.dscr("gdn_yB", [1024, T])
    pT = self.dscr("pT", [IN_COLS, T])
    ycT = self.dscr("ycT", [C_WIDTH, T], BF16)
    gn = self.vec(l, "gnorm_g")
    with Phase(kb) as ph:
        self.consts(ph)
        a_ = [ph.sb([128, 512], F32) for _ in range(2)]
        b_ = [ph.sb([128, 512], F32) for _ in range(2)]
        z_ = [ph.sb([128, 512], F32) for _ in range(2)]
        sq = ph.sb([128, 512], F32)
        o_ = [ph.sb([128, 512], BF16) for _ in range(2)]
        ps = [ph.ps([128, 512]) for _ in range(2)]
        i = 0
        for h in range(8):
            rs = slice(h * 128, (h + 1) * 128)
            for (t0, n, lat) in self.tiles:
                sl = slice(t0, t0 + n)
                ya, yb_, zz, oo, p = a_[i % 2], b_[i % 2], z_[i % 2], o_[i % 2], ps[i % 2]
                i += 1
                kb.dma("sp", ya[:, :n], yF[rs, sl], [yF], [ya], sb=ya)
                kb.dma("sp", yb_[:, :n], yB[rs, sl], [yB], [yb_], sb=yb_)
                kb.dma("sp", zz[:, :n], pT[OFF_C + 3072 + h * 128:OFF_C + 3072 + (h + 1) * 128, sl], [pT], [zz], sb=zz)
                kb.op("dve", lambda e: e.tensor_tensor(out=ya[:, :n], in0=ya[:, :n], in1=yb_[:, :n], op=ALU.add),
                      [ya, yb_], [ya])
                kb.op("act", lambda e: e.activation(out=sq[:, :n], in_=ya[:, :n], func=AF.Square), [ya], [sq])
                kb.mm(p[:, :n], self.ones[:], sq[:, :n], True, True, [self.ones, sq], [p])
                kb.op("act", lambda e: e.activation(out=yb_[:, :n], in_=p[:, :n], func=AF.Sqrt,
                                                    bias=self.eps_t[:, 0:1], scale=1.0 / C_HDIM), [p, self.eps_t], [yb_])
                kb.op("dve", lambda e: e.reciprocal(out=yb_[:, :n], in_=yb_[:, :n]), [yb_], [yb_])
                kb.op("dve", lambda e: e.scalar_tensor_tensor(out=ya[:, :n], in0=ya[:, :n], scalar=gn[:, 0:1],
                                                              in1=yb_[:, :n], op0=ALU.mult, op1=ALU.mult),
                      [ya, yb_, self.vecs], [ya])
                kb.op("act", lambda e: e.activation(out=zz[:, :n], in_=zz[:, :n], func=AF.Silu), [zz], [zz])
                kb.op("pool", lambda e: e.tensor_tensor(out=oo[:, :n], in0=ya[:, :n], in1=zz[:, :n], op=ALU.mult),
                      [ya, zz], [oo])
                kb.dma("sp", ycT[rs, sl], oo[:, :n], [oo], [ycT], sb=oo)


Model.phase_gdn_post = phase_gdn_post


def phase_moe(self, l, last):
    kb = self.kb
    self.load_vecs()
    T = self.T
    xs = self.dscr("xs", [D, T])
    xsv = xs.t.rearrange("(kc p) t -> p kc t", p=128)
    rw_d = self.din("router_w", [DEPTH, D, N_EXP])
    rb_d = self.din("rbB", [DEPTH, 128, N_EXP])
    w1_d = self.din("exp_w1", [DEPTH, N_EXP, D, 2 * E_FF])
    w2_d = self.din("exp_w2", [DEPTH, N_EXP, E_FF, D])
    b2_d = self.din("exp_b2", [DEPTH, N_EXP, D])
    ident_d = self.din("ident", [128, 128])
    selA_d = self.din("selA", [128, 128, 128])
    mod = self.mod[l]
    b1 = self.vec(l, "b1")
    tiles = [t for t in _subtiles(self.tiles, 256) if t[2] or not last]
    with Phase(kb) as ph:
        self.consts(ph)
        ident = ph.sb([128, 128], F32)
        kb.dma("sp", ident[:], ident_d[:], [ident_d], [ident], sb=ident)
        s32 = ph.sb([32, 32, 128], BF16)
        kb.dma("pool", s32[:], selA_d[0:32, 0:32, :], [selA_d], [s32], sb=s32)
        rw = ph.sb([128, 32, N_EXP], F32)
        kb.dma("sp", rw[:], rw_d.t[l].rearrange("(kc p) e -> p kc e", p=128), [rw_d], [rw], sb=rw)
        rb = ph.sb([128, N_EXP], F32)
        kb.dma("sp", rb[:], rb_d.t[l], [rb_d], [rb], sb=rb)
        b2 = ph.sb([32, D], BF16)
        kb.dma("pool", b2[:], b2_d.t[l], [b2_d], [b2], sb=b2)
        gm = ph.sb([128, 32, 2], F32)
        g2n = self.vec(l, "norm2_g")
        kb.op("dve", lambda e: e.scalar_tensor_tensor(
            out=gm[:], in0=mod[:, 128:160, :], scalar=1.0, in1=g2n.unsqueeze(2).to_broadcast([128, 32, 2]),
            op0=ALU.add, op1=ALU.mult), [mod, self.vecs], [gm])
        xa = ph.sb([128, 32, 256], F32)
        h2T = ph.sb([128, 32, 256], BF16)
        wA = ph.sb([128, 32, E_FF], BF16)
        wB = ph.sb([128, 32, E_FF], BF16)
        w2e = ph.sb([128, 4, D], BF16)
        actE = [ph.sb([128, 4, 256], BF16) for _ in range(2)]
        sq = [ph.sb([128, 256], F32) for _ in range(2)]
        rstd = ph.sb([128, 256], F32)
        tp = [ph.sb([128, 256], F32) for _ in range(2)]
        hf = [ph.sb([128, 256], F32) for _ in range(2)]
        lg = ph.sb([128, N_EXP], F32)
        m8 = ph.sb([128, 8], F32)
        nmx = ph.sb([128, 1], F32)
        msk = ph.sb([128, N_EXP], F32)
        ssum = ph.sb([128, 1], F32)
        combT = ph.sb([32, 256], F32)
        combTb = ph.sb([32, 256], BF16)
        gts = [ph.sb([128, 256], F32) for _ in range(4)]
        usb = [ph.sb([128, 256], F32) for _ in range(2)]
        sgb = [ph.sb([128, 256], F32) for _ in range(2)]
        xo = [ph.sb([128, 256], F32) for _ in range(2)]
        ps_ss = ph.ps([128, 512])
        ps_r = [ph.ps([128, 512]) for _ in range(2)]
        ps_b = ph.ps([128, 512])
        ps_m = [ph.ps([128, 512]) for _ in range(4)]
        rot = 0
        for (t0, n, lat) in tiles:
            r = 0 if lat else 1
            nsub = n // 128
            kb.dma("sp", xa[:, :, :n], xsv[:, :, t0:t0 + n], [xs], [xa], sb=xa)
            self.rms_stats(ph, lambda kc: xa[:, kc, :n], 32, n, D, self.ones, sq, ps_ss, rstd, [xa])
            for kc in range(32):
                t_, h_ = tp[kc % 2], hf[kc % 2]
                kb.op("dve", lambda e: e.scalar_tensor_tensor(
                    out=t_[:, :n], in0=xa[:, kc, :n], scalar=gm[:, kc, r:r + 1], in1=rstd[:, :n],
                    op0=ALU.mult, op1=ALU.mult), [xa, gm, rstd], [t_])
                kb.op("act", lambda e: e.activation(out=h_[:, :n], in_=t_[:, :n], func=AF.Identity,
                                                    bias=mod[:, 96 + kc, r:r + 1], scale=1.0), [t_, mod], [h_])
                kb.op("pool", lambda e: e.tensor_copy(out=h2T[:, kc, :n], in_=h_[:, :n]), [h_], [h2T])
                for sb_ in range(nsub):
                    kb.mm(ps_r[sb_][:, 0:N_EXP], h_[:, sb_ * 128:(sb_ + 1) * 128], rw[:, kc, :], kc == 0, kc == 31,
                          [h_, rw], [ps_r[sb_]])
            for sb_ in range(nsub):
                kb.op("dve", lambda e: e.tensor_tensor(out=lg[:], in0=ps_r[sb_][:, 0:N_EXP], in1=rb[:], op=ALU.add),
                      [ps_r[sb_], rb], [lg])
                kb.op("dve", lambda e: e.max(out=m8[:], in_=lg[:]), [lg], [m8])
                kb.op("dve", lambda e: e.tensor_scalar(out=msk[:], in0=lg[:], scalar1=m8[:, 3:4], scalar2=None,
                                                       op0=ALU.is_ge), [lg, m8], [msk])
                kb.op("dve", lambda e: e.tensor_scalar(out=nmx[:], in0=m8[:, 0:1], scalar1=-1.0, scalar2=None,
                                                       op0=ALU.mult), [m8], [nmx])
                kb.op("act", lambda e: e.activation(out=lg[:], in_=lg[:], func=AF.Exp, bias=nmx[:, 0:1], scale=1.0),
                      [lg, nmx], [lg])
                kb.op("dve", lambda e: e.tensor_tensor(out=lg[:], in0=lg[:], in1=msk[:], op=ALU.mult), [lg, msk], [lg])
                kb.op("dve", lambda e: e.tensor_reduce(out=ssum[:], in_=lg[:], axis=AX.X, op=ALU.add), [lg], [ssum])
                kb.op("dve", lambda e: e.reciprocal(out=ssum[:], in_=ssum[:]), [ssum], [ssum])
                kb.op("dve", lambda e: e.tensor_scalar(out=lg[:], in0=lg[:], scalar1=ssum[:, 0:1], scalar2=None,
                                                       op0=ALU.mult), [lg, ssum], [lg])
                kb.op("pe", lambda e: e.transpose(ps_b[0:32, 0:128], lg[:], ident[:]), [lg, ident], [ps_b])
                kb.op("act", lambda e: e.copy(out=combT[:, sb_ * 128:(sb_ + 1) * 128], in_=ps_b[0:32, 0:128]),
                      [ps_b], [combT])
            kb.op("dve", lambda e: e.tensor_copy(out=combTb[:, :n], in_=combT[:, :n]), [combT], [combTb])
            for m_ in range(32):
                p = ps_m[rot % 4]
                rot += 1
                kb.mm(p[:, :n], b2[:, m_ * 128:(m_ + 1) * 128], combTb[:, :n], True, True, [b2, combTb], [p])
                _evac(kb, m_, xa[:, m_, :n], p[:, :n], [p], [xa])
            for ex in range(N_EXP):
                kb.mm(ps_b[:, :n], s32[:, ex, :], combTb[:, :n], True, True, [s32, combTb], [ps_b])
                w1v = w1_d.t[l, ex].rearrange("(kc p) c -> p kc c", p=128)
                kb.dma("pool", wA[:], w1v[:, :, 0:E_FF], [w1_d], [wA], sb=wA)
                kb.dma("pool", wB[:], w1v[:, :, E_FF:2 * E_FF], [w1_d], [wB], sb=wB)
                ae = actE[ex % 2]
                for f in range(4):
                    pg = ps_m[rot % 4]
                    rot += 1
                    for kc in range(32):
                        kb.mm(pg[:, :n], wA[:, kc, f * 128:(f + 1) * 128], h2T[:, kc, :n], kc == 0, kc == 31,
                              [wA, h2T], [pg])
                    bg = b1[:, ex * 8 + f:ex * 8 + f + 1]
                    gt_ = gts[f]
                    kb.op("dve", lambda e: e.tensor_scalar(out=gt_[:, :n], in0=pg[:, :n], scalar1=bg, scalar2=LIMIT,
                                                           op0=ALU.add, op1=ALU.min), [pg, self.vecs], [gt_])
                    s_ = sgb[f % 2]
                    kb.op("act", lambda e: e.activation(out=s_[:, :n], in_=gt_[:, :n], func=AF.Sigmoid, scale=ALPHA),
                          [gt_], [s_])
                    kb.op("dve", lambda e: e.tensor_tensor(out=gt_[:, :n], in0=gt_[:, :n], in1=s_[:, :n], op=ALU.mult),
                          [gt_, s_], [gt_])
                for f in range(4):
                    pu = ps_m[rot % 4]
                    rot += 1
                    for kc in range(32):
                        kb.mm(pu[:, :n], wB[:, kc, f * 128:(f + 1) * 128], h2T[:, kc, :n], kc == 0, kc == 31,
                              [wB, h2T], [pu])
                    bu = b1[:, ex * 8 + 4 + f:ex * 8 + 4 + f + 1]
                    u_ = usb[f % 2]
                    gt_ = gts[f]
                    kb.op("dve", lambda e: e.tensor_scalar(out=u_[:, :n], in0=pu[:, :n], scalar1=bu, scalar2=LIMIT,
                                                           op0=ALU.add, op1=ALU.min), [pu, self.vecs], [u_])
                    kb.op("dve", lambda e: e.tensor_scalar(out=u_[:, :n], in0=u_[:, :n], scalar1=-LIMIT, scalar2=1.0,
                                                           op0=ALU.max, op1=ALU.add), [u_], [u_])
                    kb.op("dve", lambda e: e.tensor_tensor(out=u_[:, :n], in0=u_[:, :n], in1=gt_[:, :n], op=ALU.mult),
                          [u_, gt_], [u_])
                    kb.op("dve", lambda e: e.tensor_tensor(out=ae[:, f, :n], in0=u_[:, :n], in1=ps_b[:, :n],
                                                           op=ALU.mult), [u_, ps_b], [ae])
                kb.dma("pool", w2e[:], w2_d.t[l, ex].rearrange("(f p) c -> p f c", p=128), [w2_d], [w2e], sb=w2e)
                for m_ in range(32):
                    p = ps_m[rot % 4]
                    rot += 1
                    for f in range(4):
                        kb.mm(p[:, :n], w2e[:, f, m_ * 128:(m_ + 1) * 128], ae[:, f, :n], f == 0, f == 3, [w2e, ae], [p])
                    kb.op("dve", lambda e: e.tensor_tensor(out=xa[:, m_, :n], in0=xa[:, m_, :n], in1=p[:, :n],
                                                           op=ALU.add), [xa, p], [xa])
            for m_ in range(32):
                cs = slice(m_ * 128, (m_ + 1) * 128)
                x_ = xo[m_ % 2]
                kb.dma("sp", x_[:, :n], xs[cs, t0:t0 + n], [xs], [x_], sb=x_)
                kb.op("dve", lambda e: e.scalar_tensor_tensor(
                    out=x_[:, :n], in0=xa[:, m_, :n], scalar=mod[:, 160 + m_, r:r + 1], in1=x_[:, :n],
                    op0=ALU.mult, op1=ALU.add), [xa, mod, x_], [x_])
                kb.dma("sp", xs[cs, t0:t0 + n], x_[:, :n], [x_], [xs], sb=x_)


Model.phase_moe = phase_moe


def build_full():
    m = Model(TC_FULL, TL_FULL)
    m.phase_copy_in()
    for l in range(DEPTH):
        last = l == DEPTH - 1
        m.phase_mod(l)
        m.phase_proj(l)
        m.phase_mla(l, last)
        m.phase_rwkv_prep(l)
        m.phase_scan("rwkv", "rkvX", "rkv_vT", "rkv_y")
        m.phase_rwkv_post(l)
        m.phase_gdn_prep(l)
        m.phase_scan("gdn", "gdnX", "gdn_vT", "gdn_y")
        m.phase_gdn_post(l)
        m.phase_merge(l, last)
        m.phase_moe(l, last)
    m.phase_final()
    m.kb.final_wait()
    return m


def kernel(**inputs):
    p = {k: np.asarray(v) for k, v in inputs.items()}
    m = build_full()
    C, S, PT = rope_tables(TL_FULL)
    ident, selA, selR = host_consts()
    vecs = pack_vecs({k: p[k] for k in VEC_SRC})
    fng = _col(p["final_norm_g"], 32)
    rbB = np.ascontiguousarray(np.broadcast_to(p["router_b"][:, None, :], (DEPTH, 128, N_EXP)))
    in_maps = []
    for b in range(BATCH):
        x0 = np.ascontiguousarray(np.concatenate([p["ctx"][b], p["x"][b]], 0).T.astype(np.float32))
        csT = np.ascontiguousarray(np.stack([p["c"][b], p["c_ctx"]], 0).reshape(2, 32, 128).transpose(2, 1, 0))
        feeds = {"x0": x0, "csT": csT, "vecs": vecs, "ropeC": C, "ropeS": S, "ropePT": PT, "fng": fng,
                 "ident": ident, "selA": selA, "selR": selR, "rbB": rbB, "antiI": anti_identity(),
                 "ada_w": p["ada_w"], "w_in": p["w_in"], "mla_wqb": p["mla_wqb"], "mla_wkvb": p["mla_wkvb"],
                 "rwkv_w2": p["rwkv_w2"], "rwkv_a2": p["rwkv_a2"], "rwkv_g2": p["rwkv_g2"],
                 "w_up_a": p["w_up_a"], "w_up_b": p["w_up_b"], "w_up_c": p["w_up_c"], "w_out": p["w_out"],
                 "router_w": p["router_w"], "exp_w1": p["exp_w1"], "exp_w2": p["exp_w2"], "exp_b2": p["exp_b2"]}
        in_maps.append({k: np.ascontiguousarray(feeds[k], dtype=np.float32) for k in m.ins})
    res = run_bass_kernel_spmd(m.nc, in_maps, core_ids=list(range(BATCH)))
    out = np.stack([np.ascontiguousarray(r["outT"].T) for r in res.results], 0)
    return out.astype(np.float32)
```
